# Optimizing a Trainium2 kernel written in Bass

```python
import jax
import jax.numpy as jnp
from jax import lax
import numpy as np

D_MODEL = 2048
BATCH = 16
SEQ = 2048
DEPTH = 4

BRANCH_WIDTH = D_MODEL // 2
N_BRANCH = 3
HEAD_DIM = 64
N_Q_HEADS = BRANCH_WIDTH // HEAD_DIM
N_KV_HEADS = 4
GQA_GROUP = N_Q_HEADS // N_KV_HEADS
WINDOW = 128
BLOCK = 128
ATTN_WIDTH = N_Q_HEADS * HEAD_DIM
KV_WIDTH = N_KV_HEADS * HEAD_DIM
CONV_WIDTH = BRANCH_WIDTH
CONV_KERNEL = 31
N_MEM = 256
MEM_HEADS = 4
MEM_HEAD_DIM = BRANCH_WIDTH // MEM_HEADS
MEM_WIDTH = MEM_HEADS * MEM_HEAD_DIM
IN_SPLITS = (ATTN_WIDTH,
             ATTN_WIDTH + KV_WIDTH,
             ATTN_WIDTH + 2 * KV_WIDTH,
             ATTN_WIDTH + 2 * KV_WIDTH + 2 * CONV_WIDTH,
             ATTN_WIDTH + 2 * KV_WIDTH + 2 * CONV_WIDTH + MEM_WIDTH)
IN_WIDTH = IN_SPLITS[-1] + N_BRANCH * D_MODEL
N_GROUPS = 4
EXPERTS_PER_GROUP = 8
N_EXPERTS = N_GROUPS * EXPERTS_PER_GROUP
TOP_K_IN_GROUP = 2
D_EXPERT = D_MODEL // 4
ALPHA = (2.0 * DEPTH) ** 0.25
BETA = (8.0 * DEPTH) ** -0.25
LN_EPS = 1e-5
NEG_INF = -1e30

kernel_name = "hybrid_swa_conformer_mem_hmoe_deepnorm"


def layer_norm(x, g, b):
    x32 = x.astype(jnp.float32)
    mu = jnp.mean(x32, axis=-1, keepdims=True)
    var = jnp.mean(jnp.square(x32 - mu), axis=-1, keepdims=True)
    return ((x32 - mu) * lax.rsqrt(var + LN_EPS) * g + b).astype(x.dtype)


def alibi_slopes(n_heads):
    h = jnp.arange(1, n_heads + 1, dtype=jnp.float32)
    return jnp.exp2(-8.0 * h / n_heads)


def sliding_window_attention(q, k, v, sinks):
    b, s = q.shape[0], q.shape[1]
    nb = s // BLOCK
    qb = q.reshape(b, nb, BLOCK, N_KV_HEADS, GQA_GROUP, HEAD_DIM).astype(jnp.float32)

    def with_prev(t):
        tb = t.reshape(b, nb, BLOCK, N_KV_HEADS, HEAD_DIM).astype(jnp.float32)
        prev = jnp.pad(tb[:, :-1], ((0, 0), (1, 0), (0, 0), (0, 0), (0, 0)))
        return jnp.concatenate([prev, tb], axis=2)

    kb, vb = with_prev(k), with_prev(v)
    scores = jnp.einsum('bnqkgd,bnskd->bnkgqs', qb, kb) * (HEAD_DIM ** -0.5)
    qi = jnp.arange(BLOCK)[:, None] + BLOCK
    si = jnp.arange(2 * BLOCK)[None, :]
    dist = qi - si
    key_pos = jnp.arange(nb)[:, None, None] * BLOCK - BLOCK + si[None]
    valid = (dist >= 0)[None] & (dist < WINDOW)[None] & (key_pos >= 0)
    slopes = alibi_slopes(N_Q_HEADS).reshape(N_KV_HEADS, GQA_GROUP)
    bias = -slopes[:, :, None, None] * dist.astype(jnp.float32)[None, None]
    scores = jnp.where(valid[None, :, None, None], scores + bias[None, None], NEG_INF)
    sink = sinks.astype(jnp.float32).reshape(1, 1, N_KV_HEADS, GQA_GROUP, 1, 1)
    m = jnp.maximum(jnp.max(scores, axis=-1, keepdims=True), sink)
    p = jnp.exp(scores - m)
    probs = p / (jnp.sum(p, axis=-1, keepdims=True) + jnp.exp(sink - m))
    out = jnp.einsum('bnkgqs,bnskd->bnqkgd', probs, vb)
    return out.reshape(b, s, ATTN_WIDTH).astype(q.dtype)


def conformer_conv(u, w_dw, b_dw, ln_g, ln_b):
    a, gate = jnp.split(u, 2, axis=-1)
    h = a * jax.nn.sigmoid(gate)
    h = lax.conv_general_dilated(
        h, w_dw[:, None, :].astype(h.dtype), window_strides=(1,),
        padding=((CONV_KERNEL - 1, 0),),
        dimension_numbers=('NWC', 'WIO', 'NWC'),
        feature_group_count=CONV_WIDTH) + b_dw
    h = layer_norm(h, ln_g, ln_b)
    return jax.nn.silu(h)


def memory_attention(qm, mem_n, w_kv):
    b, s = qm.shape[0], qm.shape[1]
    km, vm = jnp.split(mem_n @ w_kv, 2, axis=-1)
    km = km.reshape(b, N_MEM, MEM_HEADS, MEM_HEAD_DIM).astype(jnp.float32)
    vm = vm.reshape(b, N_MEM, MEM_HEADS, MEM_HEAD_DIM).astype(jnp.float32)
    qh = qm.reshape(b, s, MEM_HEADS, MEM_HEAD_DIM).astype(jnp.float32)
    sc = jnp.einsum('bshd,bmhd->bhsm', qh, km) * (MEM_HEAD_DIM ** -0.5)
    p = jax.nn.softmax(sc, axis=-1)
    out = jnp.einsum('bhsm,bmhd->bshd', p, vm)
    return out.reshape(b, s, MEM_WIDTH).astype(qm.dtype)


def hierarchical_moe(x, wg, bg, we, be, w1, w3, w2):
    b, s, d = x.shape
    t = x.reshape(b * s, d)
    g_logits = (t @ wg).astype(jnp.float32) + bg
    g_prob = jax.nn.softmax(g_logits, axis=-1)
    _, grp = lax.top_k(g_logits, 1)
    p_grp = jnp.take_along_axis(g_prob, grp, axis=-1)
    e_logits = ((t @ we).astype(jnp.float32) + be).reshape(-1, N_GROUPS, EXPERTS_PER_GROUP)
    e_in = jnp.take_along_axis(e_logits, grp[:, :, None], axis=1)[:, 0]
    top_v, top_i = lax.top_k(e_in, TOP_K_IN_GROUP)
    w_top = jax.nn.softmax(top_v, axis=-1) * p_grp
    eidx = grp * EXPERTS_PER_GROUP + top_i
    comb = jnp.sum(jax.nn.one_hot(eidx, N_EXPERTS, dtype=jnp.float32) * w_top[..., None], axis=1)
    out = jnp.zeros(t.shape, jnp.float32)
    for e in range(N_EXPERTS):
        h = jax.nn.silu(t @ w1[e]) * (t @ w3[e])
        out = out + comb[:, e:e + 1] * (h @ w2[e])
    return out.reshape(b, s, d).astype(x.dtype)


def setup_inputs(seed: int = 0) -> dict:
    key = jax.random.key(seed)
    ks = jax.random.split(key, 24)
    L = DEPTH

    def nrm(k, shape, scale):
        return jax.random.normal(k, shape, jnp.float32) * scale

    return {
        'x': nrm(ks[0], (BATCH, SEQ, D_MODEL), 1.0),
        'mem': nrm(ks[1], (BATCH, N_MEM, D_MODEL), 1.0),
        'mem_ln_g': 1.0 + nrm(ks[2], (D_MODEL,), 0.02),
        'mem_ln_b': nrm(ks[3], (D_MODEL,), 0.02),
        'w_in': nrm(ks[4], (L, D_MODEL, IN_WIDTH), D_MODEL ** -0.5),
        'attn_sinks': nrm(ks[5], (L, N_Q_HEADS), 0.5),
        'conv_dw': nrm(ks[6], (L, CONV_KERNEL, CONV_WIDTH), CONV_KERNEL ** -0.5),
        'conv_dw_b': nrm(ks[7], (L, CONV_WIDTH), 0.02),
        'conv_ln_g': 1.0 + nrm(ks[8], (L, CONV_WIDTH), 0.02),
        'conv_ln_b': nrm(ks[9], (L, CONV_WIDTH), 0.02),
        'w_mem_kv': nrm(ks[10], (L, D_MODEL, 2 * MEM_WIDTH), D_MODEL ** -0.5),
        'w_branch': nrm(ks[11], (L, N_BRANCH, BRANCH_WIDTH, D_MODEL), BRANCH_WIDTH ** -0.5),
        'w_out': nrm(ks[12], (L, D_MODEL, D_MODEL), BETA * D_MODEL ** -0.5),
        'ln1_g': 1.0 + nrm(ks[13], (L, D_MODEL), 0.02),
        'ln1_b': nrm(ks[14], (L, D_MODEL), 0.02),
        'router_group': nrm(ks[15], (L, D_MODEL, N_GROUPS), D_MODEL ** -0.5),
        'router_group_b': nrm(ks[16], (L, N_GROUPS), 0.01),
        'router_expert': nrm(ks[17], (L, D_MODEL, N_EXPERTS), D_MODEL ** -0.5),
        'router_expert_b': nrm(ks[18], (L, N_EXPERTS), 0.01),
        'w1': nrm(ks[19], (L, N_EXPERTS, D_MODEL, D_EXPERT), D_MODEL ** -0.5),
        'w3': nrm(ks[20], (L, N_EXPERTS, D_MODEL, D_EXPERT), D_MODEL ** -0.5),
        'w2': nrm(ks[21], (L, N_EXPERTS, D_EXPERT, D_MODEL), BETA * D_EXPERT ** -0.5),
        'ln2_g': 1.0 + nrm(ks[22], (L, D_MODEL), 0.02),
        'ln2_b': nrm(ks[23], (L, D_MODEL), 0.02),
    }


def reference(x, mem, mem_ln_g, mem_ln_b, w_in, attn_sinks, conv_dw, conv_dw_b,
              conv_ln_g, conv_ln_b, w_mem_kv, w_branch, w_out, ln1_g, ln1_b,
              router_group, router_group_b, router_expert, router_expert_b,
              w1, w3, w2, ln2_g, ln2_b):
    b, s = x.shape[0], x.shape[1]
    mem_n = layer_norm(mem, mem_ln_g, mem_ln_b)
    for l in range(DEPTH):
        proj = x @ w_in[l]
        q, k, v, u, qm, gates = jnp.split(proj, IN_SPLITS, axis=-1)
        q = q.reshape(b, s, N_Q_HEADS, HEAD_DIM)
        k = k.reshape(b, s, N_KV_HEADS, HEAD_DIM)
        v = v.reshape(b, s, N_KV_HEADS, HEAD_DIM)
        o_attn = sliding_window_attention(q, k, v, attn_sinks[l])
        o_conv = conformer_conv(u, conv_dw[l], conv_dw_b[l], conv_ln_g[l], conv_ln_b[l])
        o_mem = memory_attention(qm, mem_n, w_mem_kv[l])
        g = jax.nn.sigmoid(gates.astype(jnp.float32)).reshape(b, s, N_BRANCH, D_MODEL)
        merged = g[:, :, 0] * (o_attn @ w_branch[l, 0])
        merged = merged + g[:, :, 1] * (o_conv @ w_branch[l, 1])
        merged = merged + g[:, :, 2] * (o_mem @ w_branch[l, 2])
        y = merged.astype(x.dtype) @ w_out[l]
        x = layer_norm(ALPHA * x + y, ln1_g[l], ln1_b[l])
        f = hierarchical_moe(x, router_group[l], router_group_b[l], router_expert[l],
                             router_expert_b[l], w1[l], w3[l], w2[l])
        x = layer_norm(ALPHA * x + f, ln2_g[l], ln2_b[l])
    return x
```

```python
import contextlib
import numpy as np
import concourse.bass as bass
import concourse.mybir as mybir
from concourse.bass_utils import run_bass_kernel_spmd

F32 = mybir.dt.float32
BF16 = mybir.dt.bfloat16
I32 = mybir.dt.int32
AF = mybir.ActivationFunctionType
ALU = mybir.AluOpType
AX = mybir.AxisListType

ENGS = ("pe", "act", "dve", "pool", "sp")
NDMA_SLOTS = 12


class Op:
    __slots__ = ("eng", "fn", "deps", "dma", "slot", "slot_val", "sig", "sig_val", "prev_slot_op")

    def __init__(self, eng, fn, dma):
        self.eng = eng
        self.fn = fn
        self.deps = []
        self.dma = dma
        self.sig = False
        self.sig_val = 0
        self.slot = None
        self.slot_val = 0
        self.prev_slot_op = None


class Sched:
    def __init__(self, nc):
        self.nc = nc
        self.q = {e: [] for e in ENGS}
        self.last_w = {}
        self.readers = {}
        self.dma_rr = {e: 0 for e in ENGS}
        self.slot_last = {}
        self.slot_cnt = {}

    def op(self, eng, fn, reads=(), writes=(), dma=False):
        o = Op(eng, fn, dma)
        deps = set()
        for r in reads:
            w = self.last_w.get(r)
            if w is not None:
                deps.add(w)
        for wr in writes:
            w = self.last_w.get(wr)
            if w is not None:
                deps.add(w)
            for rd in self.readers.get(wr, ()):
                deps.add(rd)
        for d in deps:
            if (not dma) and (not d.dma) and d.eng == eng:
                if eng == "pe":
                    continue
                raw = False
                for r in reads:
                    if self.last_w.get(r) is d:
                        raw = True
                        break
                if not raw:
                    continue
            o.deps.append(d)
        for r in reads:
            self.readers.setdefault(r, []).append(o)
        for wr in writes:
            self.last_w[wr] = o
            self.readers[wr] = []
        if dma:
            s = self.dma_rr[eng]
            self.dma_rr[eng] = (s + 1) % NDMA_SLOTS
            key = (eng, s)
            o.slot = key
            o.prev_slot_op = self.slot_last.get(key)
            self.slot_cnt[key] = self.slot_cnt.get(key, 0) + 1
            o.slot_val = 16 * self.slot_cnt[key]
            self.slot_last[key] = o
        self.q[eng].append(o)
        return o

    def barrier(self):
        lasts = []
        for e in ENGS:
            for o in reversed(self.q[e]):
                if not o.dma and o.fn is not None:
                    lasts.append(o)
                    break
        dmas = list(self.slot_last.values())
        for e in ENGS:
            o = Op(e, None, False)
            o.deps = list(lasts) + dmas
            self.q[e].append(o)
        self.last_w = {}
        self.readers = {}

    def emit(self):
        nc = self.nc
        for e in ENGS:
            for o in self.q[e]:
                for d in o.deps:
                    if not d.dma:
                        d.sig = True
        cnt = {e: 0 for e in ENGS}
        for e in ENGS:
            for o in self.q[e]:
                if o.sig and not o.dma:
                    cnt[e] += 1
                    o.sig_val = cnt[e]
        with contextlib.ExitStack() as st:
            esem = {e: st.enter_context(nc.semaphore("s_" + e)) for e in ENGS}
            dsem = {}
            for key in self.slot_cnt:
                dsem[key] = st.enter_context(nc.semaphore("d_%s_%d" % key))
            block = st.enter_context(nc.Block())

            def run(e, eng):
                waited = {}

                def wait(sem_key, sem, val):
                    if waited.get(sem_key, 0) >= val:
                        return
                    waited[sem_key] = val
                    eng.wait_ge(sem, val)

                for o in self.q[e]:
                    for d in o.deps:
                        if d.dma:
                            wait(d.slot, dsem[d.slot], d.slot_val)
                        else:
                            wait(d.eng, esem[d.eng], d.sig_val)
                    if o.dma and o.prev_slot_op is not None:
                        p = o.prev_slot_op
                        wait(p.slot, dsem[p.slot], p.slot_val)
                    if o.fn is None:
                        continue
                    ins = o.fn(eng)
                    if o.dma:
                        ins.then_inc(dsem[o.slot], 16)
                    elif o.sig:
                        ins.then_inc(esem[e], 1)
                for key, last in self.slot_last.items():
                    if key[0] == e:
                        wait(key, dsem[key], last.slot_val)

            block.tensor(lambda eng: run("pe", eng))
            block.scalar(lambda eng: run("act", eng))
            block.vector(lambda eng: run("dve", eng))
            block.gpsimd(lambda eng: run("pool", eng))
            block.sync(lambda eng: run("sp", eng))


D = 2048
SEQ = 2048
NMEM = 256
INW = 10752
ST = 1024
NBLK = ST // 128
Q0, K0, V0, UA0, UG0, QM0, G0 = 0, 1024, 1280, 1536, 2560, 3584, 4608
ALPHA = (2.0 * 4) ** 0.25
LN_EPS = 1e-5
NEXP = 32
WUNIT = 1024
NWUNIT = 20

PARAM_SPECS = [
    ("mem_ln_g", (D,)), ("mem_ln_b", (D,)),
    ("w_in", ("L", D, INW)), ("attn_sinks", ("L", 16)), ("conv_dw", ("L", 31, 1024)),
    ("conv_dw_b", ("L", 1024)), ("conv_ln_g", ("L", 1024)), ("conv_ln_b", ("L", 1024)),
    ("w_mem_kv", ("L", D, 2048)), ("w_branch", ("L", 3, 1024, D)), ("w_out", ("L", D, D)),
    ("ln1_g", ("L", D)), ("ln1_b", ("L", D)),
    ("router_group", ("L", D, 4)), ("router_group_b", ("L", 4)),
    ("router_expert", ("L", D, 32)), ("router_expert_b", ("L", 32)),
    ("w1", ("L", NEXP, D, 512)), ("w3", ("L", NEXP, D, 512)), ("w2", ("L", NEXP, 512, D)),
    ("ln2_g", ("L", D)), ("ln2_b", ("L", D)),
]


class Builder:
    def __init__(self, NS=2, NL=4, debug=None, moe=True):
        self.NS, self.NL = NS, NL
        self.T = NS * SEQ
        self.NST = self.T // ST
        self.debug = debug or {}
        self.moe = moe
        nc = self.nc = bass.Bass("TRN2", target_bir_lowering=False)
        self.S = Sched(nc)
        T = self.T
        dt = nc.dram_tensor
        self.x_d = dt("x", [T, D], F32, kind="ExternalInput").ap()
        self.mem_d = dt("mem", [NS * NMEM, D], F32, kind="ExternalInput").ap()
        self.p = {}
        for name, shp in PARAM_SPECS:
            shp = [NL if s == "L" else s for s in shp]
            self.p[name] = dt(name, shp, F32, kind="ExternalInput").ap()
        self.out_d = dt("out", [T, D], F32, kind="ExternalOutput").ap()
        self.dbg_d = {}
        for name, shp in self.debug.items():
            if name in ("substop", "att_stage", "att_iters"):
                continue
            self.dbg_d[name] = dt(name, list(shp), F32, kind="ExternalOutput").ap()
        self.XTd = dt("XTd", [D, T], BF16).ap()
        self.XRd = dt("XRd", [T, D], F32).ap()
        self.KMd = dt("KMd", [NL, 128, 8, NS * NMEM], BF16).ap()
        self.VMd = dt("VMd", [NL, 128, NS * 2, 1024], BF16).ap()
        self.WCH = 640000
        self.WCTOT = 2 * self.WCH
        self.WCs = [dt("WC%d" % i, [128, self.WCH], BF16).ap() for i in range(2)]
        self.cache_mode = None
        self.cache_off = 0
        sb = nc.alloc_sbuf_tensor
        self.XT = sb("XT", [128, 16, ST], BF16).ap()
        self.M = sb("M", [128, 25600], F32).ap()
        self.Wring = sb("Wring", [128, WUNIT * NWUNIT], BF16).ap()
        self.LNG = sb("LNG", [128, D], F32).ap()
        self.LNB = sb("LNB", [128, D], F32).ap()
        self.identb = sb("identb", [128, 128], BF16).ap()
        self.identf = sb("identf", [128, 128], F32).ap()
        self.onesb = sb("onesb", [128, 128], BF16).ap()
        self.onesf = sb("onesf", [128, 128], F32).ap()
        self.ND = sb("ND", [128, 2, 128], F32).ap()
        self.es2 = sb("es2", [128, NL, 8], F32).ap()
        self.dw = sb("dw", [128, NL, 8, 31], F32).ap()
        self.dwb = sb("dwb", [128, NL, 8], F32).ap()
        self.cg = sb("cg", [128, NL, 8], F32).ap()
        self.cb = sb("cb", [128, NL, 8], F32).ap()
        self.Wr = sb("Wr", [128, 16, 36], BF16).ap()
        self.rb = sb("rb", [128, 36], F32).ap()
        self.kcarry = sb("kcarry", [128, 4, 128], BF16).ap()
        self.vcarry = sb("vcarry", [128, 256], BF16).ap()
        self.hcarry = sb("hcarry", [128, 8, 32], BF16).ap()
        self.small = sb("small", [128, 64], F32).ap()
        self.stage = self.mview(65536, [128, 1024], F32)
        self.PS = [nc.alloc_psum_tensor("PS%d" % i, [128, 512], F32).ap() for i in range(8)]
        self.wpos = 0

    def mview(self, off_bytes, shape, dtype):
        assert off_bytes % 4 == 0
        n = int(np.prod(shape[1:]))
        esz = 2 if dtype == BF16 else 4
        nf32 = (n * esz + 3) // 4
        v = self.M[:, off_bytes // 4: off_bytes // 4 + nf32]
        if dtype != F32:
            v = v.bitcast(dtype)
        v = v[:, 0:n]
        if len(shape) == 2:
            return v
        names = " ".join("d%d" % i for i in range(1, len(shape)))
        kw = {"d%d" % i: shape[i] for i in range(1, len(shape))}
        return v.rearrange("p (%s) -> p %s" % (names, names), **kw)

    def walloc(self, kc, ncols):
        n = kc * ncols
        nu = (n + WUNIT - 1) // WUNIT
        if self.wpos + nu > NWUNIT:
            self.wpos = 0
        u0 = self.wpos
        self.wpos += nu
        view = self.Wring[:, u0 * WUNIT: u0 * WUNIT + n].rearrange("p (k n) -> p k n", k=kc)
        return view, [("W", u) for u in range(u0, u0 + nu)]

    def wcache(self, view, regs, n, loaders):
        flat = view.rearrange("p k n -> p (k n)")
        if self.cache_off < self.WCH and self.cache_off + n > self.WCH:
            self.cache_off = self.WCH
        off = self.cache_off
        if self.cache_mode is not None:
            self.cache_off += n
            assert self.cache_off <= self.WCTOT
        wi_ = off // self.WCH
        img = self.WCs[wi_][:, off - wi_ * self.WCH: off - wi_ * self.WCH + n]
        if self.cache_mode == "use":
            self.S.op("sp", lambda e: e.dma_start(out=flat, in_=img), reads=[("WC", off)], writes=regs, dma=True)
            return
        for fn in loaders:
            self.S.op("pool", fn, writes=regs, dma=True)
        if self.cache_mode == "fill":
            self.S.op("sp", lambda e: e.dma_start(out=img, in_=flat), reads=regs, writes=[("WC", off)], dma=True)

    def load_w(self, src, kc, ncols):
        view, regs = self.walloc(kc, ncols)
        self.wcache(view, regs, kc * ncols,
                    [lambda e: e.dma_start(out=view, in_=src.rearrange("(k p) n -> p k n", p=128))])
        return view, regs

    def mm(self, out, pairs, reads, writes):
        def f(e):
            n = len(pairs)
            ins = None
            for i, (l, r) in enumerate(pairs):
                ins = e.matmul(out, l, r, start=(i == 0), stop=(i == n - 1))
            return ins
        return self.S.op("pe", f, reads=reads, writes=writes)

    def dbg(self, name, src, reads):
        if name in self.dbg_d:
            self.S.op("sp", lambda e: e.dma_start(out=self.dbg_d[name], in_=src), reads=reads,
                      writes=[("dbg", name)], dma=True)

    def consts(self):
        S = self.S
        identf, identb = self.identf, self.identb
        S.op("pool", lambda e: e.memset(identf, 0.0), writes=["identf"])
        S.op("pool", lambda e: e.affine_select(identf, identf, [[-1, 128]], ALU.not_equal, 1.0, base=0,
                                               channel_multiplier=1), reads=["identf"], writes=["identf"])
        S.op("dve", lambda e: e.tensor_copy(identb, identf), reads=["identf"], writes=["identb"])
        S.op("dve", lambda e: e.memset(self.onesb, 1.0), writes=["onesb"])
        S.op("dve", lambda e: e.memset(self.onesf, 1.0), writes=["onesf"])
        vi = self.mview(0, [128, 128], I32)
        vf = self.mview(1024, [128, 128], F32)
        vf2 = self.mview(2048, [128, 128], F32)
        S.op("pool", lambda e: e.iota(vi, [[-1, 128]], base=0, channel_multiplier=1), writes=["vi"])
        S.op("dve", lambda e: e.tensor_copy(vf, vi), reads=["vi"], writes=["vf"])
        S.op("dve", lambda e: e.tensor_scalar_add(vf2, vf, -128.0), reads=["vf"], writes=["vf2"])
        ND = self.ND
        S.op("pool", lambda e: e.affine_select(ND[:, 1, :], vf, [[1, 128]], ALU.is_ge, -1e9, base=0,
                                               channel_multiplier=-1), reads=["vf"], writes=["ND1"])
        S.op("pool", lambda e: e.affine_select(ND[:, 0, :], vf2, [[-1, 128]], ALU.is_ge, -1e9, base=-1,
                                               channel_multiplier=1), reads=["vf2"], writes=["ND0"])
        NL = self.NL
        stage = self.stage
        for l in range(NL):
            sk = self.small[:, 0:16]
            S.op("sp", lambda e, l=l: e.dma_start(out=sk, in_=self.p["attn_sinks"][l:l + 1, :].partition_broadcast(128)),
                 writes=["sk"], dma=True)
            S.op("act", lambda e: e.activation(sk, sk, AF.Exp), reads=["sk"], writes=["sk"])
            skv = sk.rearrange("p (c two) -> p c two", two=2)
            S.op("dve", lambda e, l=l: e.tensor_copy(self.es2[0:64, l, :], skv[0:64, :, 0]), reads=["sk"], writes=[("es2", l, 0)])
            S.op("dve", lambda e, l=l: e.tensor_copy(self.es2[64:128, l, :], skv[64:128, :, 1]), reads=["sk"], writes=[("es2", l, 1)])
            S.op("sp", lambda e, l=l: e.dma_start(out=stage[0:31, :], in_=self.p["conv_dw"][l]), writes=["stage"], dma=True)
            pt = self.PS[0][:, 0:248].rearrange("p (c j) -> p c j", c=8)

            def tr(e):
                ins = None
                for c in range(8):
                    ins = e.transpose(pt[:, c, :], stage[0:31, c * 128:(c + 1) * 128], identf[0:31, 0:31])
                return ins
            S.op("pe", tr, reads=["stage", "identf"], writes=["PS0"])
            S.op("dve", lambda e, l=l: e.tensor_copy(self.dw[:, l, :, :], pt), reads=["PS0"], writes=[("dw", l)])
            for j, (nm, dst) in enumerate((("conv_dw_b", self.dwb), ("conv_ln_g", self.cg), ("conv_ln_b", self.cb))):
                S.op("sp", lambda e, l=l, nm=nm, j=j: e.dma_start(
                    out=stage[32 + 8 * j: 40 + 8 * j, 0:128],
                    in_=self.p[nm][l].rearrange("(c p) -> c p", p=128)), writes=[("stv", j)], dma=True)
            pv = self.PS[1][:, 0:24]
            S.op("pe", lambda e: e.transpose(pv, stage[32:56, 0:128], identf[32:56, 32:56]),
                 reads=[("stv", 0), ("stv", 1), ("stv", 2), "identf"], writes=["PS1"])
            for j, dst in enumerate((self.dwb, self.cg, self.cb)):
                S.op("dve", lambda e, l=l, j=j, dst=dst: e.tensor_copy(dst[:, l, :], pv[:, 8 * j: 8 * j + 8]),
                     reads=["PS1"], writes=[("vec", l, j)])
        S.barrier()

    def load_ln(self, gname, bname, l=None):
        S = self.S
        g = self.p[gname]
        b = self.p[bname]
        gsrc = (g[l:l + 1, :] if l is not None else g.rearrange("(o d) -> o d", o=1)).partition_broadcast(128)
        bsrc = (b[l:l + 1, :] if l is not None else b.rearrange("(o d) -> o d", o=1)).partition_broadcast(128)
        S.op("sp", lambda e: e.dma_start(out=self.LNG, in_=gsrc), writes=["LNG"], dma=True)
        S.op("sp", lambda e: e.dma_start(out=self.LNB, in_=bsrc), writes=["LNB"], dma=True)

    def layer_norm_block(self, xr, key, stats_off, small=False):
        S = self.S
        if small:
            base = self.small[:, (stats_off // 128) * 32:(stats_off // 128) * 32 + 32]
            st = base[:, 0:24].rearrange("p (a b) -> p a b", a=4)
            mv = base[:, 24:26]
            rs = base[:, 26:28]
        else:
            st = self.mview(stats_off, [128, 4, 6], F32)
            mv = self.mview(stats_off + 96, [128, 2], F32)
            rs = self.mview(stats_off + 104, [128, 2], F32)
        skey = ("lnst", stats_off)

        def stats(e):
            ins = None
            for i in range(4):
                ins = e.bn_stats(st[:, i, :], xr[:, i * 512:(i + 1) * 512])
            return ins
        S.op("dve", stats, reads=[key], writes=[skey + (0,)])
        S.op("dve", lambda e: e.bn_aggr(mv, st), reads=[skey + (0,)], writes=[skey + (1,)])
        S.op("dve", lambda e: e.tensor_scalar_add(rs[:, 0:1], mv[:, 1:2], LN_EPS), reads=[skey + (1,)], writes=[skey + (2,)])
        S.op("act", lambda e: e.activation(rs[:, 0:1], rs[:, 0:1], AF.Sqrt), reads=[skey + (2,)], writes=[skey + (2,)])
        S.op("dve", lambda e: e.reciprocal(rs[:, 0:1], rs[:, 0:1]), reads=[skey + (2,)], writes=[skey + (3,)])
        S.op("dve", lambda e: e.tensor_scalar(rs[:, 1:2], mv[:, 0:1], rs[:, 0:1], -1.0, ALU.mult, ALU.mult),
             reads=[skey + (1,), skey + (3,)], writes=[skey + (4,)])
        S.op("act", lambda e: e.activation(xr, xr, AF.Identity, bias=rs[:, 1:2], scale=rs[:, 0:1]),
             reads=[key, skey + (3,), skey + (4,)], writes=[key])
        S.op("dve", lambda e: e.tensor_mul(xr, xr, self.LNG), reads=[key, "LNG"], writes=[key])
        S.op("dve", lambda e: e.tensor_add(xr, xr, self.LNB), reads=[key, "LNB"], writes=[key])

    def transpose_block(self, xb, xb_key, dstT, dst_col0, dst_keys, ps_pair=(4, 5)):
        S = self.S
        for hh in range(2):
            ps = self.PS[ps_pair[hh]].bitcast(BF16)[:, 0:1024].rearrange("p (k t) -> p k t", k=8)
            pkey = "PS%d" % ps_pair[hh]

            def tr(e, hh=hh, ps=ps):
                ins = None
                for k in range(8):
                    kk = hh * 8 + k
                    ins = e.transpose(ps[:, k, :], xb[:, kk * 128:(kk + 1) * 128], self.identb)
                return ins
            S.op("pe", tr, reads=[xb_key, "identb"], writes=[pkey])
            eng = "act" if hh == 0 else "dve"
            dk = dst_keys(hh) if callable(dst_keys) else dst_keys
            if eng == "act":
                S.op("act", lambda e, hh=hh, ps=ps: e.copy(dstT[:, hh * 8:(hh + 1) * 8, dst_col0:dst_col0 + 128], ps),
                     reads=[pkey], writes=dk)
            else:
                S.op("dve", lambda e, hh=hh, ps=ps: e.tensor_copy(dstT[:, hh * 8:(hh + 1) * 8, dst_col0:dst_col0 + 128], ps),
                     reads=[pkey], writes=dk)

    def prologue_mem(self):
        S = self.S
        NS = self.NS
        nmb = NS * 2
        self.load_ln("mem_ln_g", "mem_ln_b")
        memT = self.XT
        for mb in range(nmb):
            xr = self.mview(mb % 2 * 8192, [128, D], F32)
            key = ("memxr", mb % 2)
            S.op("sp", lambda e, mb=mb, xr=xr: e.dma_start(out=xr, in_=self.mem_d[mb * 128:(mb + 1) * 128, :]), writes=[key], dma=True)
            self.layer_norm_block(xr, key, (mb % 2) * 128, small=True)
            xb = self.mview(16384 + (mb % 2) * 4096, [128, D], BF16)
            xkey = ("memxb", mb % 2)
            S.op("act", lambda e, xb=xb, xr=xr: e.copy(xb, xr), reads=[key], writes=[xkey])
            self.transpose_block(xb, xkey, memT, mb * 128, [("XT", mb)])
        ncol = nmb * 128
        for l in range(self.NL):
            kmT = self.mview(32768, [128, 8, ncol], BF16)
            vm = self.mview(32768 + 8192, [128, nmb, 1024], BF16)
            xtk = [("XT", mb) for mb in range(nmb)]
            for ct in range(8):
                wt, wreg = self.load_w(self.p["w_mem_kv"][l][:, ct * 256:(ct + 1) * 256], 16, 256)
                if ct < 4:
                    for half in range(2):
                        c = ct * 2 + half
                        ps = self.PS[c % 2]
                        self.mm(ps[:, 0:ncol], [(wt[:, k, half * 128:(half + 1) * 128], memT[:, k, 0:ncol]) for k in range(16)],
                                reads=wreg + xtk, writes=["PS%d" % (c % 2)])
                        S.op("act", lambda e, c=c, ps=ps: e.copy(kmT[:, c, :], ps[:, 0:ncol]), reads=["PS%d" % (c % 2)], writes=[("kmT", c)])
                else:
                    vc = ct - 4
                    for mb in range(nmb):
                        ps = self.PS[2 + mb % 2]
                        self.mm(ps[:, 0:256], [(memT[:, k, mb * 128:(mb + 1) * 128], wt[:, k, :]) for k in range(16)],
                                reads=wreg + xtk, writes=["PS%d" % (2 + mb % 2)])
                        S.op("dve", lambda e, mb=mb, vc=vc, ps=ps: e.tensor_copy(vm[:, mb, vc * 256:(vc + 1) * 256], ps[:, 0:256]),
                             reads=["PS%d" % (2 + mb % 2)], writes=[("vm", mb, vc)])
            S.op("sp", lambda e, l=l: e.dma_start(out=self.KMd[l], in_=kmT), reads=[("kmT", c) for c in range(8)], writes=[("KMd", l)], dma=True)
            S.op("sp", lambda e, l=l: e.dma_start(out=self.VMd[l], in_=vm), reads=[("vm", mb, vc) for mb in range(nmb) for vc in range(4)],
                 writes=[("VMd", l)], dma=True)
        S.barrier()

    def prologue_xT(self):
        S = self.S
        XTd_v = self.XTd.rearrange("(k p) t -> p k t", p=128)
        for s in range(self.NST):
            for b in range(NBLK):
                row0 = s * ST + b * 128
                xr = self.mview((b % 2) * 8192, [128, D], F32)
                key = ("pxr", b % 2)
                S.op("sp", lambda e, xr=xr, row0=row0: e.dma_start(out=xr, in_=self.x_d[row0:row0 + 128, :]), writes=[key], dma=True)
                xb = self.mview(16384 + (b % 2) * 4096, [128, D], BF16)
                xkey = ("pxb", b % 2)
                S.op("act", lambda e, xb=xb, xr=xr: e.copy(xb, xr), reads=[key], writes=[xkey])
                self.transpose_block(xb, xkey, self.XT, b * 128, [("XT", b)])
            S.op("sp", lambda e, s=s: e.dma_start(out=XTd_v[:, :, s * ST:(s + 1) * ST], in_=self.XT),
                 reads=[("XT", b) for b in range(NBLK)], writes=[("XTd", s)], dma=True)
        S.barrier()

    def load_XT(self, s):
        XTd_v = self.XTd.rearrange("(k p) t -> p k t", p=128)
        for hh in range(2):
            self.S.op("sp", lambda e, hh=hh: e.dma_start(out=self.XT[:, hh * 8:(hh + 1) * 8, :],
                                                         in_=XTd_v[:, hh * 8:(hh + 1) * 8, s * ST:(s + 1) * ST]),
                      reads=[("XTd", s)], writes=[("XTh", hh)], dma=True)
        self.xt_keys = [("XTh", 0), ("XTh", 1)]

    def proj_fm(self, l, col0, ntile, evac, ps_ids=(0, 1), wsrc=None):
        w = self.p["w_in"][l] if wsrc is None else wsrc
        i = 0
        for ct in range(ntile):
            wt, wreg = self.load_w(w[:, col0 + ct * 256: col0 + (ct + 1) * 256], 16, 256)
            for half in range(2):
                c = ct * 2 + half
                for tt in range(2):
                    pid = ps_ids[i % len(ps_ids)]
                    i += 1
                    ps = self.PS[pid]
                    self.mm(ps, [(wt[:, k, half * 128:(half + 1) * 128], self.XT[:, k, tt * 512:(tt + 1) * 512]) for k in range(16)],
                            reads=wreg + self.xt_keys, writes=["PS%d" % pid])
                    evac(c, tt, ps, "PS%d" % pid)

    def phase_B(self, l, s):
        S = self.S
        seq_first = (s % (SEQ // ST) == 0)
        h = self.mview(0, [128, 8, 1056], BF16)
        diag = [self.mview(16896 + i * 7936, [128, 31, 128], BF16) for i in range(2)]
        sgt = [self.mview(32768 + i * 2048, [128, 512], F32) for i in range(2)]
        sqt = [self.mview(36864 + i * 2048, [128, 512], F32) for i in range(2)]
        stt = [[self.mview(40960 + (tt * 3 + j) * 2048, [128, 512], F32) for j in range(3)] for tt in range(2)]
        cv = self.mview(53248, [128, 8, ST], F32)
        oc = self.mview(86016, [128, 8, ST], BF16)
        if seq_first:
            S.op("dve", lambda e: e.memset(h[:, :, 0:32], 0.0), writes=[("hh", c) for c in range(8)])
        else:
            S.op("dve", lambda e: e.tensor_copy(h[:, :, 0:32], self.hcarry), reads=["hcarry"], writes=[("hh", c) for c in range(8)])
        wi = self.p["w_in"][l]
        cnt = 0
        for pc in range(4):
            wa, wareg = self.load_w(wi[:, UA0 + pc * 256: UA0 + (pc + 1) * 256], 16, 256)
            wg, wgreg = self.load_w(wi[:, UG0 + pc * 256: UG0 + (pc + 1) * 256], 16, 256)
            for half in range(2):
                c = pc * 2 + half
                for tt in range(2):
                    pa, pg = cnt % 2, 2 + cnt % 2
                    sg = sgt[cnt % 2]
                    sgk = ("sgt", cnt % 2)
                    cnt += 1
                    xs = self.XT
                    self.mm(self.PS[pa], [(wa[:, k, half * 128:(half + 1) * 128], xs[:, k, tt * 512:(tt + 1) * 512]) for k in range(16)],
                            reads=wareg + self.xt_keys, writes=["PS%d" % pa])
                    self.mm(self.PS[pg], [(wg[:, k, half * 128:(half + 1) * 128], xs[:, k, tt * 512:(tt + 1) * 512]) for k in range(16)],
                            reads=wgreg + self.xt_keys, writes=["PS%d" % pg])
                    S.op("act", lambda e, sg=sg, pg=pg: e.activation(sg, self.PS[pg], AF.Sigmoid), reads=["PS%d" % pg], writes=[sgk])
                    S.op("dve", lambda e, sg=sg, pa=pa, c=c, tt=tt: e.tensor_tensor(h[:, c, 32 + tt * 512: 32 + (tt + 1) * 512], self.PS[pa], sg, ALU.mult),
                         reads=["PS%d" % pa, sgk], writes=[("h", c, tt)])
        S.op("dve", lambda e: e.tensor_copy(self.hcarry, h[:, :, 1024:1056]), reads=[("h", c, 1) for c in range(8)], writes=["hcarry"])
        cnt = 0
        for c in range(8):
            dg = diag[c % 2]
            dgk = ("diag", c % 2)
            S.op("pool", lambda e, dg=dg, c=c: e.tensor_tensor(dg, self.identb.unsqueeze(1).to_broadcast([128, 31, 128]),
                                                              self.dw[:, l, c, :].unsqueeze(2).to_broadcast([128, 31, 128]), ALU.mult),
                 reads=["identb", ("dw", l)], writes=[dgk])
            for tt in range(2):
                pid = 4 + cnt % 2
                sq = sqt[cnt % 2]
                sqk = ("sqt", cnt % 2)
                cnt += 1
                rd = [dgk, ("hh", c), ("h", c, tt)] + ([("h", c, 0)] if tt == 1 else [])
                self.mm(self.PS[pid], [(dg[:, j, :], h[:, c, 2 + j + tt * 512: 2 + j + tt * 512 + 512]) for j in range(31)],
                        reads=rd, writes=["PS%d" % pid])
                cvs = cv[:, c, tt * 512:(tt + 1) * 512]
                S.op("act", lambda e, cvs=cvs, pid=pid, c=c: e.activation(cvs, self.PS[pid], AF.Identity, bias=self.dwb[:, l, c:c + 1]),
                     reads=["PS%d" % pid, ("vec", l, 0)], writes=[("cv", c, tt)])
                S.op("pool", lambda e, cvs=cvs, sq=sq: e.tensor_tensor(sq, cvs, cvs, ALU.mult), reads=[("cv", c, tt)], writes=[sqk])
                S.op("pe", lambda e, cvs=cvs, tt=tt, c=c: e.matmul(self.PS[tt * 2], self.onesf, cvs, start=(c == 0), stop=(c == 7)),
                     reads=[("cv", c, tt), "onesf"], writes=["PS%d" % (tt * 2)])
                S.op("pe", lambda e, sq=sq, tt=tt, c=c: e.matmul(self.PS[tt * 2 + 1], self.onesf, sq, start=(c == 0), stop=(c == 7)),
                     reads=[sqk, "onesf"], writes=["PS%d" % (tt * 2 + 1)])
        for tt in range(2):
            mean, rstd, tmp = stt[tt]
            k0 = ("stt", tt)
            S.op("dve", lambda e, mean=mean, tt=tt: e.tensor_scalar_mul(mean, self.PS[tt * 2], 1.0 / 1024), reads=["PS%d" % (tt * 2)], writes=[k0 + (0,)])
            S.op("dve", lambda e, tmp=tmp, mean=mean: e.tensor_tensor(tmp, mean, mean, ALU.mult), reads=[k0 + (0,)], writes=[k0 + (2,)])
            S.op("dve", lambda e, rstd=rstd, tmp=tmp, tt=tt: e.scalar_tensor_tensor(rstd, self.PS[tt * 2 + 1], 1.0 / 1024, tmp, ALU.mult, ALU.subtract),
                 reads=["PS%d" % (tt * 2 + 1), k0 + (2,)], writes=[k0 + (1,)])
            S.op("dve", lambda e, rstd=rstd: e.tensor_scalar_add(rstd, rstd, LN_EPS), reads=[k0 + (1,)], writes=[k0 + (1,)])
            S.op("act", lambda e, rstd=rstd: e.activation(rstd, rstd, AF.Sqrt), reads=[k0 + (1,)], writes=[k0 + (1,)])
            S.op("dve", lambda e, rstd=rstd: e.reciprocal(rstd, rstd), reads=[k0 + (1,)], writes=[k0 + (1,)])
            for c in range(8):
                cvs = cv[:, c, tt * 512:(tt + 1) * 512]
                S.op("dve", lambda e, cvs=cvs, mean=mean: e.tensor_tensor(cvs, cvs, mean, ALU.subtract), reads=[("cv", c, tt), k0 + (0,)], writes=[("cv", c, tt)])
                S.op("pool", lambda e, cvs=cvs, rstd=rstd: e.tensor_tensor(cvs, cvs, rstd, ALU.mult), reads=[("cv", c, tt), k0 + (1,)], writes=[("cv", c, tt)])
                S.op("act", lambda e, cvs=cvs, c=c, tt=tt: e.activation(oc[:, c, tt * 512:(tt + 1) * 512], cvs, AF.Silu,
                                                                        bias=self.cb[:, l, c:c + 1], scale=self.cg[:, l, c:c + 1]),
                     reads=[("cv", c, tt), ("vec", l, 1), ("vec", l, 2)], writes=[("oc", c, tt)])
        if "oc" in self.dbg_d and s == 0 and l == 0:
            S.barrier()
            tmpf = self.mview(0, [128, 8, ST], F32)
            S.op("dve", lambda e: e.tensor_copy(tmpf, oc), writes=["tmpf"])
            self.dbg("oc", tmpf, ["tmpf"])
        S.barrier()

    def phase_A(self, l, s):
        S = self.S
        seq_first = (s % (SEQ // ST) == 0)
        qT = self.mview(0, [128, 8, ST], BF16)
        kd = self.mview(16384, [128, 4, 1152], BF16)
        v = self.mview(25600, [128, 9, 256], BF16)
        tmps = [self.mview(30208 + i * 2048, [128, 2, 2, 128], F32) for i in range(2)]
        pTs = [self.mview(34304 + i * 1024, [128, 2, 2, 128], BF16) for i in range(2)]
        recs = [self.mview(36352 + i * 512, [128, 128], F32) for i in range(2)]
        oa = self.mview(53248, [128, 8, ST], BF16)
        wi = self.p["w_in"][l]
        if not seq_first:
            S.op("dve", lambda e: e.tensor_copy(kd[:, :, 0:128], self.kcarry), reads=["kcarry"], writes=[("kd", g, -1) for g in range(4)])
            S.op("dve", lambda e: e.tensor_copy(v[:, 0, :], self.vcarry), reads=["vcarry"], writes=[("v", -1)])
        def evq(c, tt, ps, pk):
            if (c + tt) % 2 == 0:
                S.op("act", lambda e: e.copy(qT[:, c, tt * 512:(tt + 1) * 512], ps), reads=[pk], writes=[("qT", c, tt * 4 + i) for i in range(4)])
            else:
                S.op("dve", lambda e: e.tensor_copy(qT[:, c, tt * 512:(tt + 1) * 512], ps), reads=[pk], writes=[("qT", c, tt * 4 + i) for i in range(4)])
        self.proj_fm(l, Q0, 4, evq)
        if self.debug.get("substop") == 1:
            S.barrier(); return
        for t in range(2):
            view, wreg = self.walloc(16, 256)
            v5 = view.rearrange("p k (g u d) -> p k g u d", g=2, u=2)
            src = wi[:, K0 + t * 128: K0 + (t + 1) * 128].rearrange("(k p) (g d) -> p k g d", p=128, g=2)
            self.wcache(view, wreg, 16 * 256,
                        [(lambda e, u=u, g=g, v5=v5, src=src: e.dma_start(out=v5[:, :, g, u, :], in_=src[:, :, g, :]))
                         for u in range(2) for g in range(2)])
            for half in range(2):
                g = t * 2 + half
                for tt in range(2):
                    pid = (half * 2 + tt) % 2
                    ps = self.PS[pid]
                    self.mm(ps, [(view[:, k, half * 128:(half + 1) * 128], self.XT[:, k, tt * 512:(tt + 1) * 512]) for k in range(16)],
                            reads=wreg + self.xt_keys, writes=["PS%d" % pid])
                    S.op("act", lambda e, g=g, tt=tt, ps=ps: e.copy(kd[:, g, 128 + tt * 512: 128 + (tt + 1) * 512], ps),
                         reads=["PS%d" % pid], writes=[("kd", g, tt * 4 + i) for i in range(4)])
        S.op("dve", lambda e: e.tensor_copy(self.kcarry, kd[:, :, 1024:1152]), reads=[("kd", g, 7) for g in range(4)], writes=["kcarry"])
        if self.debug.get("substop") == 2:
            S.barrier(); return
        wv, wvreg = self.load_w(wi[:, V0:V0 + 256], 16, 256)
        for b in range(NBLK):
            pid = b % 2
            ps = self.PS[pid]
            self.mm(ps[:, 0:256], [(self.XT[:, k, b * 128:(b + 1) * 128], wv[:, k, :]) for k in range(16)],
                    reads=wvreg + self.xt_keys, writes=["PS%d" % pid])
            S.op("dve", lambda e, b=b, ps=ps: e.tensor_copy(v[:, 1 + b, :], ps[:, 0:256]), reads=["PS%d" % pid], writes=[("v", b)])
        S.op("dve", lambda e: e.tensor_copy(self.vcarry, v[:, 8, :]), reads=[("v", 7)], writes=["vcarry"])
        if self.debug.get("substop") == 3:
            S.barrier(); return
        it = 0
        for b in range(NBLK):
            js = [1] if (seq_first and b == 0) else [0, 1]
            for c in range(8):
                g = c // 2
                i2 = it % 2
                it += 1
                pss2 = [self.PS[2 * a + i2][:, 0:256].rearrange("p (j t) -> p j t", j=2) for a in range(2)]
                pskeys = ["PS%d" % (2 * a + i2) for a in range(2)]
                pso = self.PS[4 + i2][:, 0:128]
                psd = self.PS[6 + i2][:, 0:128]
                tmp, pT, rec = tmps[i2], pTs[i2], recs[i2]

                def sc(e, b=b, c=c, g=g, js=js, pss2=pss2):
                    ins = None
                    for j in js:
                        for a in range(2):
                            ins = e.matmul(pss2[a][:, j, :], kd[a * 64:(a + 1) * 64, g, (b + j) * 128:(b + j + 1) * 128],
                                           qT[a * 64:(a + 1) * 64, c, b * 128:(b + 1) * 128], start=True, stop=True)
                    return ins
                stg = self.debug.get("att_stage", 9)
                if it > self.debug.get("att_iters", 10 ** 9):
                    continue
                S.op("pe", sc, reads=[("qT", c, b)] + [("kd", g, b - 1 + j) for j in js], writes=pskeys)
                if stg < 2:
                    continue
                for a in range(2):
                    hd = 2 * c + a
                    slope8 = 8.0 * (2.0 ** (-(hd + 1) / 2.0))
                    j0 = js[0]
                    S.op("dve", lambda e, a=a, j0=j0, slope8=slope8, tmp=tmp, pss2=pss2: e.scalar_tensor_tensor(
                        tmp[:, a, j0:2, :], self.ND[:, j0:2, :], slope8, pss2[a][:, j0:2, :], ALU.mult, ALU.add),
                        reads=[pskeys[a], "ND0", "ND1"], writes=[("tmp", i2, a)])
                j0 = js[0]
                if stg < 3:
                    continue
                S.op("act", lambda e, j0=j0, tmp=tmp, pT=pT: e.activation(pT[:, :, j0:2, :], tmp[:, :, j0:2, :], AF.Exp, scale=0.125),
                     reads=[("tmp", i2, 0), ("tmp", i2, 1)], writes=[("pT", i2)])

                if stg < 4:
                    continue

                def pv(e, b=b, g=g, js=js, pso=pso, psd=psd, pT=pT):
                    ins = None
                    for a in range(2):
                        for ji, j in enumerate(js):
                            e.matmul(pso[a * 64:(a + 1) * 64, :], v[:, b + j, g * 64:(g + 1) * 64], pT[:, a, j, :],
                                     start=(ji == 0), stop=(ji == len(js) - 1))
                    for a in range(2):
                        for ji, j in enumerate(js):
                            ins = e.matmul(psd[a * 64:(a + 1) * 64, :], self.onesb[:, 0:64], pT[:, a, j, :],
                                           start=(ji == 0), stop=(ji == len(js) - 1))
                    return ins
                S.op("pe", pv, reads=[("pT", i2), "onesb"] + [("v", b - 1 + j) for j in js], writes=["PS%d" % (4 + i2), "PS%d" % (6 + i2)])
                if stg < 5:
                    continue
                S.op("dve", lambda e, rec=rec, psd=psd, c=c: e.tensor_scalar(rec, psd, self.es2[:, l, c:c + 1], 1.0, ALU.add, ALU.mult),
                     reads=["PS%d" % (6 + i2), ("es2", l, 0), ("es2", l, 1)], writes=[("rec", i2)])
                S.op("dve", lambda e, rec=rec: e.reciprocal(rec, rec), reads=[("rec", i2)], writes=[("rec", i2)])
                S.op("dve", lambda e, rec=rec, pso=pso, c=c, b=b: e.tensor_tensor(oa[:, c, b * 128:(b + 1) * 128], pso, rec, ALU.mult),
                     reads=["PS%d" % (4 + i2), ("rec", i2)], writes=[("oa", c, b)])
        if "oa" in self.dbg_d and s == 0 and l == 0:
            S.barrier()
            tmpf = self.mview(0, [128, 8, ST], F32)
            S.op("dve", lambda e: e.tensor_copy(tmpf, oa), writes=["tmpf"])
            self.dbg("oa", tmpf, ["tmpf"])
        S.barrier()

    def phase_C(self, l, s):
        S = self.S
        seq = s // (SEQ // ST)
        qm = self.mview(0, [128, 8, ST], BF16)
        kmT = self.mview(16384, [128, 8, NMEM], BF16)
        vm = self.mview(20480, [128, 2, 1024], BF16)
        pTs = [self.mview(24576 + i * 2048, [128, 2, 512], BF16) for i in range(2)]
        recs = [self.mview(28672 + i * 2048, [128, 512], F32) for i in range(2)]
        om = self.mview(69632, [128, 8, ST], BF16)
        S.op("sp", lambda e: e.dma_start(out=kmT, in_=self.KMd[l][:, :, seq * NMEM:(seq + 1) * NMEM]), reads=[("KMd", l)], writes=["kmT"], dma=True)
        S.op("sp", lambda e: e.dma_start(out=vm, in_=self.VMd[l][:, seq * 2:(seq + 1) * 2, :]), reads=[("VMd", l)], writes=["vm"], dma=True)

        def evq(c, tt, ps, pk):
            if (c + tt) % 2 == 0:
                S.op("act", lambda e: e.copy(qm[:, c, tt * 512:(tt + 1) * 512], ps), reads=[pk], writes=[("qm", c, tt)])
            else:
                S.op("dve", lambda e: e.tensor_copy(qm[:, c, tt * 512:(tt + 1) * 512], ps), reads=[pk], writes=[("qm", c, tt)])
        self.proj_fm(l, QM0, 4, evq)
        it = 0
        for hh in range(4):
            for tt in range(2):
                i2 = it % 2
                it += 1
                pT, rec = pTs[i2], recs[i2]
                for kb in range(2):
                    pid = 2 + kb
                    self.mm(self.PS[pid], [(kmT[:, 2 * hh + d, kb * 128:(kb + 1) * 128], qm[:, 2 * hh + d, tt * 512:(tt + 1) * 512]) for d in range(2)],
                            reads=["kmT", ("qm", 2 * hh, tt), ("qm", 2 * hh + 1, tt)], writes=["PS%d" % pid])
                    S.op("act", lambda e, kb=kb, pid=pid, pT=pT: e.activation(pT[:, kb, :], self.PS[pid], AF.Exp, scale=1.0 / 16.0),
                         reads=["PS%d" % pid], writes=[("pTm", i2, kb)])
                for d in range(2):
                    pid = 4 + d
                    self.mm(self.PS[pid], [(vm[:, kb, hh * 256 + d * 128: hh * 256 + (d + 1) * 128], pT[:, kb, :]) for kb in range(2)],
                            reads=["vm", ("pTm", i2, 0), ("pTm", i2, 1)], writes=["PS%d" % pid])
                self.mm(self.PS[6], [(self.onesb, pT[:, kb, :]) for kb in range(2)],
                        reads=["onesb", ("pTm", i2, 0), ("pTm", i2, 1)], writes=["PS6"])
                S.op("dve", lambda e, rec=rec: e.reciprocal(rec, self.PS[6]), reads=["PS6"], writes=[("recm", i2)])
                for d in range(2):
                    pid = 4 + d
                    S.op("dve", lambda e, d=d, pid=pid, rec=rec, hh=hh, tt=tt: e.tensor_tensor(
                        om[:, 2 * hh + d, tt * 512:(tt + 1) * 512], self.PS[pid], rec, ALU.mult),
                        reads=["PS%d" % pid, ("recm", i2)], writes=[("om", 2 * hh + d, tt)])
        if "om" in self.dbg_d and s == 0 and l == 0:
            S.barrier()
            tmpf = self.mview(0, [128, 8, ST], F32)
            S.op("dve", lambda e: e.tensor_copy(tmpf, om), writes=["tmpf"])
            self.dbg("om", tmpf, ["tmpf"])
        S.barrier()

    def phase_D(self, l, s):
        S = self.S
        mg = self.mview(0, [128, 16, ST], BF16)
        sgs = [self.mview(32768 + i * 2048, [128, 512], F32) for i in range(3)]
        mts = [self.mview(38912 + i * 2048, [128, 512], F32) for i in range(3)]
        O = [self.mview(53248, [128, 8, ST], BF16), self.mview(86016, [128, 8, ST], BF16), self.mview(69632, [128, 8, ST], BF16)]
        wi = self.p["w_in"][l]
        wb = self.p["w_branch"][l]
        for c in range(16):
            gt = [self.load_w(wi[:, G0 + i * D + c * 128: G0 + i * D + (c + 1) * 128], 16, 128) for i in range(3)]
            bt = [self.load_w(wb[i][:, c * 128:(c + 1) * 128], 8, 128) for i in range(3)]
            for tt in range(2):
                for i in range(3):
                    self.mm(self.PS[i], [(gt[i][0][:, k, :], self.XT[:, k, tt * 512:(tt + 1) * 512]) for k in range(16)],
                            reads=gt[i][1] + self.xt_keys, writes=["PS%d" % i])
                for i in range(3):
                    self.mm(self.PS[3 + i], [(bt[i][0][:, k, :], O[i][:, k, tt * 512:(tt + 1) * 512]) for k in range(8)],
                            reads=bt[i][1], writes=["PS%d" % (3 + i)])
                for i in range(3):
                    S.op("act", lambda e, i=i: e.activation(sgs[i], self.PS[i], AF.Sigmoid), reads=["PS%d" % i], writes=[("sgs", i)])
                    S.op("dve", lambda e, i=i: e.tensor_tensor(mts[i], sgs[i], self.PS[3 + i], ALU.mult),
                         reads=[("sgs", i), "PS%d" % (3 + i)], writes=[("mts", i)])
                S.op("dve", lambda e: e.tensor_tensor(mts[0], mts[0], mts[1], ALU.add), reads=[("mts", 0), ("mts", 1)], writes=[("mts", 0)])
                S.op("dve", lambda e, c=c, tt=tt: e.tensor_tensor(mg[:, c, tt * 512:(tt + 1) * 512], mts[0], mts[2], ALU.add),
                     reads=[("mts", 0), ("mts", 2)], writes=[("mg", c, tt)])
        if "mg" in self.dbg_d and s == 0 and l == 0:
            S.barrier()
            for hh in range(2):
                tmpf = self.mview(36864, [128, 8, ST], F32)
                S.op("dve", lambda e, hh=hh: e.tensor_copy(tmpf, mg[:, hh * 8:(hh + 1) * 8, :]), reads=["tmpf"], writes=["tmpf"])
                S.op("sp", lambda e, hh=hh: e.dma_start(out=self.dbg_d["mg"][:, hh * 8:(hh + 1) * 8, :], in_=tmpf), reads=["tmpf"], writes=["tmpf"], dma=True)
        S.barrier()

    def phase_E(self, l, s):
        S = self.S
        mg = self.mview(0, [128, 16, ST], BF16)
        xb = self.mview(32768, [128, D], BF16)
        self.load_ln("ln1_g", "ln1_b", l)
        wo = self.p["w_out"][l]
        src = self.x_d if l == 0 else self.XRd
        for half in range(2):
            xr = self.mview(36864 + half * 32768, [128, 4, D], F32)
            for b4 in range(4):
                row0 = s * ST + (half * 4 + b4) * 128
                S.op("sp", lambda e, b4=b4, row0=row0, xr=xr: e.dma_start(out=xr[:, b4, :], in_=src[row0:row0 + 128, :]),
                     reads=[("XRd", s)], writes=[("xr", half, b4)], dma=True)
            for n in range(4):
                wts = [self.load_w(wo[:, n * 512 + q * 256: n * 512 + (q + 1) * 256], 16, 256) for q in range(2)]
                for b4 in range(4):
                    blk = half * 4 + b4
                    pid = (n * 4 + b4) % 4

                    def f(e, blk=blk, pid=pid, wts=wts):
                        ins = None
                        for q in range(2):
                            for k in range(16):
                                ins = e.matmul(self.PS[pid][:, q * 256:(q + 1) * 256], mg[:, k, blk * 128:(blk + 1) * 128],
                                               wts[q][0][:, k, :], start=(k == 0), stop=(k == 15))
                        return ins
                    S.op("pe", f, reads=wts[0][1] + wts[1][1], writes=["PS%d" % pid])
                    S.op("dve", lambda e, b4=b4, n=n, pid=pid, xr=xr: e.scalar_tensor_tensor(
                        xr[:, b4, n * 512:(n + 1) * 512], xr[:, b4, n * 512:(n + 1) * 512], ALPHA, self.PS[pid], ALU.mult, ALU.add),
                        reads=["PS%d" % pid, ("xr", half, b4)], writes=[("xr", half, b4)])
            for b4 in range(4):
                blk = half * 4 + b4
                key = ("xr", half, b4)
                xrb = xr[:, b4, :]
                self.layer_norm_block(xrb, key, (blk % 2) * 128, small=True)
                if "x1" in self.dbg_d and s == 0 and l == 0:
                    S.op("sp", lambda e, blk=blk, xrb=xrb: e.dma_start(out=self.dbg_d["x1"][blk * 128:(blk + 1) * 128, :], in_=xrb),
                         reads=[key], writes=[("dbgx1", blk)], dma=True)
                S.op("act", lambda e, xrb=xrb: e.copy(xb, xrb), reads=[key], writes=["xb"])
                self.transpose_block(xb, "xb", self.XT, blk * 128, lambda hh: [("XTh", hh)])
                S.op("act", lambda e, xrb=xrb: e.mul(xrb, xrb, ALPHA), reads=[key, ("dbgx1", blk)], writes=[key])
        S.barrier()

    def phase_F(self, l, s):
        S = self.S
        last = (l == self.NL - 1)
        yacc = self.mview(36864, [128, 8, D], F32)
        hg = [self.mview(i * 8192, [128, 4, ST], BF16) for i in range(2)]
        sts = [self.mview(16384 + i * 2048, [128, 512], F32) for i in range(2)]
        comb = self.mview(20480, [128, 8, 32], F32)
        lg = self.mview(21504, [128, 8, 36], F32)
        em = self.mview(22656, [128, 8, 32], F32)
        ee = self.mview(23680, [128, 8, 32], F32)
        gsh = self.mview(24704, [128, 8, 4], F32)
        ge = self.mview(24832, [128, 8, 4], F32)
        mx = self.mview(24960, [128, 8, 8], F32)
        gmx = self.mview(25216, [128, 8], F32)
        gs = self.mview(25248, [128, 8], F32)
        d21 = self.mview(25280, [128, 8], F32)
        fac = self.mview(25312, [128, 8], F32)
        xb = self.mview(28672, [128, D], BF16)
        xk = [("XTh", 0), ("XTh", 1)]
        Wr, rb = self.Wr, self.rb
        if s == 0:
            S.op("pool", lambda e: e.dma_start(out=Wr[:, :, 0:4], in_=self.p["router_group"][l].rearrange("(k p) n -> p k n", p=128)),
                 writes=["Wr0"], dma=True)
            S.op("pool", lambda e: e.dma_start(out=Wr[:, :, 4:36], in_=self.p["router_expert"][l].rearrange("(k p) n -> p k n", p=128)),
                 writes=["Wr1"], dma=True)
            S.op("sp", lambda e: e.dma_start(out=rb[:, 0:4], in_=self.p["router_group_b"][l:l + 1, :].partition_broadcast(128)), writes=["rb0"], dma=True)
            S.op("sp", lambda e: e.dma_start(out=rb[:, 4:36], in_=self.p["router_expert_b"][l:l + 1, :].partition_broadcast(128)), writes=["rb1"], dma=True)
        self.load_ln("ln2_g", "ln2_b", l)
        psl = self.PS[0][:, 0:288].rearrange("p (b n) -> p b n", b=8)

        def rl(e):
            ins = None
            for b in range(8):
                for k in range(16):
                    ins = e.matmul(psl[:, b, :], self.XT[:, k, b * 128:(b + 1) * 128], Wr[:, k, :], start=(k == 0), stop=(k == 15))
            return ins
        S.op("pe", rl, reads=["Wr0", "Wr1"] + xk, writes=["PS0"])
        S.op("dve", lambda e: e.tensor_tensor(lg, psl, rb.unsqueeze(1).to_broadcast([128, 8, 36]), ALU.add), reads=["PS0", "rb0", "rb1"], writes=["lg"])
        S.op("dve", lambda e: e.tensor_reduce(gmx, lg[:, :, 0:4], AX.X, ALU.max), reads=["lg"], writes=["gmx"])
        S.op("dve", lambda e: e.tensor_tensor(gsh, lg[:, :, 0:4], gmx.unsqueeze(2).to_broadcast([128, 8, 4]), ALU.subtract), reads=["lg", "gmx"], writes=["gsh"])
        S.op("act", lambda e: e.activation(ge, gsh, AF.Exp), reads=["gsh"], writes=["ge"])
        S.op("dve", lambda e: e.tensor_reduce(gs, ge, AX.X, ALU.add), reads=["ge"], writes=["gs"])
        S.op("dve", lambda e: e.tensor_single_scalar(ge, gsh, 0.0, ALU.is_ge), reads=["gsh", "gs"], writes=["goh"])
        S.op("dve", lambda e: e.tensor_scalar(ge, ge, 1.0, 1e30, ALU.subtract, ALU.mult), reads=["goh"], writes=["goh"])
        lge = lg[:, :, 4:36].rearrange("p b (g j) -> p b g j", g=4)
        em4 = em.rearrange("p b (g j) -> p b g j", g=4)
        S.op("dve", lambda e: e.tensor_tensor(em4, lge, ge.unsqueeze(3).to_broadcast([128, 8, 4, 8]), ALU.add), reads=["lg", "goh"], writes=["em"])

        def mx8(e):
            ins = None
            for b in range(8):
                ins = e.max(mx[:, b, :], em[:, b, :])
            return ins
        S.op("dve", mx8, reads=["em"], writes=["mx"])
        S.op("dve", lambda e: e.tensor_tensor(comb, em, mx[:, :, 1:2].to_broadcast([128, 8, 32]), ALU.is_ge), reads=["em", "mx"], writes=["sel"])
        S.op("dve", lambda e: e.tensor_tensor(ee, em, mx[:, :, 0:1].to_broadcast([128, 8, 32]), ALU.subtract), reads=["em", "mx"], writes=["ee"])
        S.op("act", lambda e: e.activation(ee, ee, AF.Exp), reads=["ee"], writes=["ee"])
        S.op("dve", lambda e: e.tensor_tensor(d21, mx[:, :, 1], mx[:, :, 0], ALU.subtract), reads=["mx"], writes=["d21"])
        S.op("act", lambda e: e.activation(d21, d21, AF.Exp), reads=["d21"], writes=["d21"])
        S.op("dve", lambda e: e.scalar_tensor_tensor(fac, d21, 1.0, gs, ALU.add, ALU.mult), reads=["d21", "gs"], writes=["fac"])
        S.op("dve", lambda e: e.reciprocal(fac, fac), reads=["fac"], writes=["fac"])
        S.op("dve", lambda e: e.tensor_tensor(comb, comb, ee, ALU.mult), reads=["sel", "ee"], writes=["sel"])
        S.op("dve", lambda e: e.tensor_tensor(comb, comb, fac.unsqueeze(2).to_broadcast([128, 8, 32]), ALU.mult), reads=["sel", "fac"], writes=["comb"])
        if "comb" in self.dbg_d and s == 0 and l == 0:
            self.dbg("comb", comb, ["comb"])
        w1, w3, w2 = self.p["w1"][l], self.p["w3"][l], self.p["w2"][l]
        cnt = 0
        cnt4 = 0
        nexp = NEXP if self.moe else 0
        for ex in range(nexp):
            hgb = hg[ex % 2]
            for t in range(2):
                w1t, w1r = self.load_w(w1[ex][:, t * 256:(t + 1) * 256], 16, 256)
                w3t, w3r = self.load_w(w3[ex][:, t * 256:(t + 1) * 256], 16, 256)
                for half in range(2):
                    hc = t * 2 + half
                    for tt in range(2):
                        pa, pb = cnt % 2, 2 + cnt % 2
                        st = sts[cnt % 2]
                        stk = ("sts", cnt % 2)
                        cnt += 1
                        self.mm(self.PS[pa], [(w1t[:, k, half * 128:(half + 1) * 128], self.XT[:, k, tt * 512:(tt + 1) * 512]) for k in range(16)],
                                reads=w1r + xk, writes=["PS%d" % pa])
                        self.mm(self.PS[pb], [(w3t[:, k, half * 128:(half + 1) * 128], self.XT[:, k, tt * 512:(tt + 1) * 512]) for k in range(16)],
                                reads=w3r + xk, writes=["PS%d" % pb])
                        S.op("act", lambda e, st=st, pa=pa: e.activation(st, self.PS[pa], AF.Silu), reads=["PS%d" % pa], writes=[stk])
                        S.op("dve", lambda e, st=st, pb=pb, hc=hc, tt=tt, hgb=hgb: e.tensor_tensor(hgb[:, hc, tt * 512:(tt + 1) * 512], st, self.PS[pb], ALU.mult),
                             reads=[stk, "PS%d" % pb], writes=[("hg", ex % 2, hc, tt)])
            for q in range(2):
                w2t, w2r = self.load_w(w2[ex][:, q * 1024:(q + 1) * 1024], 4, 1024)
                for b in range(8):
                    for n2 in range(2):
                        pid = 4 + cnt4 % 4
                        cnt4 += 1
                        self.mm(self.PS[pid], [(hgb[:, hc, b * 128:(b + 1) * 128], w2t[:, hc, n2 * 512:(n2 + 1) * 512]) for hc in range(4)],
                                reads=w2r + [("hg", ex % 2, hc, b // 4) for hc in range(4)], writes=["PS%d" % pid])
                        ys = yacc[:, b, q * 1024 + n2 * 512: q * 1024 + (n2 + 1) * 512]
                        S.op("dve", lambda e, ys=ys, pid=pid, b=b, ex=ex: e.scalar_tensor_tensor(ys, self.PS[pid], comb[:, b, ex:ex + 1], ys, ALU.mult, ALU.add),
                             reads=["PS%d" % pid, "comb", ("yacc", b)], writes=[("yacc", b)])
        dst = self.out_d if last else self.XRd
        for b in range(8):
            key = ("yacc", b)
            yb = yacc[:, b, :]
            self.layer_norm_block(yb, key, (b % 2) * 128, small=True)
            row0 = s * ST + b * 128
            S.op("sp", lambda e, yb=yb, row0=row0: e.dma_start(out=dst[row0:row0 + 128, :], in_=yb), reads=[key], writes=[("XRd", s, b)], dma=True)
            if not last:
                S.op("act", lambda e, yb=yb: e.copy(xb, yb), reads=[key], writes=["xb"])
                self.transpose_block(xb, "xb", self.XT, b * 128, lambda hh: [("XTh", hh)])
        if not last:
            XTd_v = self.XTd.rearrange("(k p) t -> p k t", p=128)
            S.op("sp", lambda e: e.dma_start(out=XTd_v[:, :, s * ST:(s + 1) * ST], in_=self.XT), reads=xk, writes=[("XTd", s)], dma=True)
        S.barrier()

    def build(self, stop=None):
        def fin():
            self.S.emit()
            return self.nc
        self.consts()
        if stop == "consts":
            return fin()
        self.prologue_mem()
        if stop == "mem":
            return fin()
        self.prologue_xT()
        if stop == "xT":
            return fin()
        for l in range(self.NL):
            for s in range(self.NST):
                self.cache_mode = "fill" if s == 0 else "use"
                if self.NST == 1:
                    self.cache_mode = None
                self.cache_off = 0
                self.load_XT(s)
                for nm, ph in (("B", self.phase_B), ("A", self.phase_A), ("C", self.phase_C), ("D", self.phase_D),
                               ("E", self.phase_E), ("F", self.phase_F)):
                    ph(l, s)
                    if stop == nm:
                        return fin()
        return fin()


def make_in_maps(inputs, n_cores, NS, NL):
    x = np.ascontiguousarray(inputs["x"], dtype=np.float32)
    mem = np.ascontiguousarray(inputs["mem"], dtype=np.float32)
    params = {}
    for name, shp in PARAM_SPECS:
        a = np.asarray(inputs[name], dtype=np.float32)
        if shp[0] == "L":
            a = a[:NL]
        params[name] = np.ascontiguousarray(a)
    maps = []
    for c in range(n_cores):
        m = dict(params)
        m["x"] = np.ascontiguousarray(x[c * NS:(c + 1) * NS].reshape(NS * SEQ, D))
        m["mem"] = np.ascontiguousarray(mem[c * NS:(c + 1) * NS].reshape(NS * NMEM, D))
        maps.append(m)
    return maps


def kernel(**inputs):
    n_cores = 8
    NS, NL = 2, 4
    nc = Builder(NS=NS, NL=NL).build()
    in_maps = make_in_maps(inputs, n_cores, NS, NL)
    res = run_bass_kernel_spmd(nc, in_maps, core_ids=list(range(n_cores)))
    outs = [np.asarray(r["out"]).reshape(NS, SEQ, D) for r in res.results]
    return np.concatenate(outs, axis=0).astype(np.float32)
```

```python
import contextlib
import numpy as np
import concourse.bass as bass
import concourse.mybir as mybir
from concourse.bass_utils import run_bass_kernel_spmd

F32 = mybir.dt.float32
BF16 = mybir.dt.bfloat16
I32 = mybir.dt.int32
AF = mybir.ActivationFunctionType
ALU = mybir.AluOpType
AX = mybir.AxisListType

ENGS = ("pe", "act", "dve", "pool", "sp")
NDMA_SLOTS = 12


class Op:
    __slots__ = ("eng", "fn", "deps", "dma", "slot", "slot_val", "sig", "sig_val", "prev_slot_op")

    def __init__(self, eng, fn, dma):
        self.eng = eng
        self.fn = fn
        self.deps = []
        self.dma = dma
        self.sig = False
        self.sig_val = 0
        self.slot = None
        self.slot_val = 0
        self.prev_slot_op = None


class Sched:
    def __init__(self, nc):
        self.nc = nc
        self.q = {e: [] for e in ENGS}
        self.last_w = {}
        self.readers = {}
        self.dma_rr = {e: 0 for e in ENGS}
        self.slot_last = {}
        self.slot_cnt = {}

    def op(self, eng, fn, reads=(), writes=(), dma=False):
        o = Op(eng, fn, dma)
        deps = set()
        for r in reads:
            w = self.last_w.get(r)
            if w is not None:
                deps.add(w)
        for wr in writes:
            w = self.last_w.get(wr)
            if w is not None:
                deps.add(w)
            for rd in self.readers.get(wr, ()):
                deps.add(rd)
        for d in deps:
            if (not dma) and (not d.dma) and d.eng == eng:
                if eng == "pe":
                    continue
                raw = False
                for r in reads:
                    if self.last_w.get(r) is d:
                        raw = True
                        break
                if not raw:
                    continue
            o.deps.append(d)
        for r in reads:
            self.readers.setdefault(r, []).append(o)
        for wr in writes:
            self.last_w[wr] = o
            self.readers[wr] = []
        if dma:
            s = self.dma_rr[eng]
            self.dma_rr[eng] = (s + 1) % NDMA_SLOTS
            key = (eng, s)
            o.slot = key
            o.prev_slot_op = self.slot_last.get(key)
            self.slot_cnt[key] = self.slot_cnt.get(key, 0) + 1
            o.slot_val = 16 * self.slot_cnt[key]
            self.slot_last[key] = o
        self.q[eng].append(o)
        return o

    def barrier(self):
        lasts = []
        for e in ENGS:
            for o in reversed(self.q[e]):
                if not o.dma and o.fn is not None:
                    lasts.append(o)
                    break
        dmas = list(self.slot_last.values())
        for e in ENGS:
            o = Op(e, None, False)
            o.deps = list(lasts) + dmas
            self.q[e].append(o)
        self.last_w = {}
        self.readers = {}

    def emit(self):
        nc = self.nc
        for e in ENGS:
            for o in self.q[e]:
                for d in o.deps:
                    if not d.dma:
                        d.sig = True
        cnt = {e: 0 for e in ENGS}
        for e in ENGS:
            for o in self.q[e]:
                if o.sig and not o.dma:
                    cnt[e] += 1
                    o.sig_val = cnt[e]
        with contextlib.ExitStack() as st:
            esem = {e: st.enter_context(nc.semaphore("s_" + e)) for e in ENGS}
            dsem = {}
            for key in self.slot_cnt:
                dsem[key] = st.enter_context(nc.semaphore("d_%s_%d" % key))
            block = st.enter_context(nc.Block())

            def run(e, eng):
                waited = {}

                def wait(sem_key, sem, val):
                    if waited.get(sem_key, 0) >= val:
                        return
                    waited[sem_key] = val
                    eng.wait_ge(sem, val)

                for o in self.q[e]:
                    for d in o.deps:
                        if d.dma:
                            wait(d.slot, dsem[d.slot], d.slot_val)
                        else:
                            wait(d.eng, esem[d.eng], d.sig_val)
                    if o.dma and o.prev_slot_op is not None:
                        p = o.prev_slot_op
                        wait(p.slot, dsem[p.slot], p.slot_val)
                    if o.fn is None:
                        continue
                    ins = o.fn(eng)
                    if o.dma:
                        ins.then_inc(dsem[o.slot], 16)
                    elif o.sig:
                        ins.then_inc(esem[e], 1)
                for key, last in self.slot_last.items():
                    if key[0] == e:
                        wait(key, dsem[key], last.slot_val)

            block.tensor(lambda eng: run("pe", eng))
            block.scalar(lambda eng: run("act", eng))
            block.vector(lambda eng: run("dve", eng))
            block.gpsimd(lambda eng: run("pool", eng))
            block.sync(lambda eng: run("sp", eng))


D = 2048
SEQ = 2048
NMEM = 256
INW = 10752
ST = 1024
NBLK = ST // 128
Q0, K0, V0, UA0, UG0, QM0, G0 = 0, 1024, 1280, 1536, 2560, 3584, 4608
ALPHA = (2.0 * 4) ** 0.25
LN_EPS = 1e-5
NEXP = 32
WUNIT = 1024
NWUNIT = 20

PARAM_SPECS = [
    ("mem_ln_g", (D,)), ("mem_ln_b", (D,)),
    ("w_in", ("L", D, INW)), ("attn_sinks", ("L", 16)), ("conv_dw", ("L", 31, 1024)),
    ("conv_dw_b", ("L", 1024)), ("conv_ln_g", ("L", 1024)), ("conv_ln_b", ("L", 1024)),
    ("w_mem_kv", ("L", D, 2048)), ("w_branch", ("L", 3, 1024, D)), ("w_out", ("L", D, D)),
    ("ln1_g", ("L", D)), ("ln1_b", ("L", D)),
    ("router_group", ("L", D, 4)), ("router_group_b", ("L", 4)),
    ("router_expert", ("L", D, 32)), ("router_expert_b", ("L", 32)),
    ("w1", ("L", NEXP, D, 512)), ("w3", ("L", NEXP, D, 512)), ("w2", ("L", NEXP, 512, D)),
    ("ln2_g", ("L", D)), ("ln2_b", ("L", D)),
]


class Builder:
    def __init__(self, NS=2, NL=4, debug=None, moe=True):
        self.NS, self.NL = NS, NL
        self.T = NS * SEQ
        self.NST = self.T // ST
        self.debug = debug or {}
        self.moe = moe
        nc = self.nc = bass.Bass("TRN2", target_bir_lowering=False)
        self.S = Sched(nc)
        T = self.T
        dt = nc.dram_tensor
        self.x_d = dt("x", [T, D], F32, kind="ExternalInput").ap()
        self.mem_d = dt("mem", [NS * NMEM, D], F32, kind="ExternalInput").ap()
        self.p = {}
        for name, shp in PARAM_SPECS:
            shp = [NL if s == "L" else s for s in shp]
            self.p[name] = dt(name, shp, F32, kind="ExternalInput").ap()
        self.out_d = dt("out", [T, D], F32, kind="ExternalOutput").ap()
        self.dbg_d = {}
        for name, shp in self.debug.items():
            if name in ("substop", "att_stage", "att_iters"):
                continue
            self.dbg_d[name] = dt(name, list(shp), F32, kind="ExternalOutput").ap()
        self.XTd = dt("XTd", [D, T], BF16).ap()
        self.XRd = dt("XRd", [T, D], F32).ap()
        self.KMd = dt("KMd", [NL, 128, 8, NS * NMEM], BF16).ap()
        self.VMd = dt("VMd", [NL, 128, NS * 2, 1024], BF16).ap()
        self.WCH = 640000
        self.WCTOT = 2 * self.WCH
        self.WCs = [dt("WC%d" % i, [128, self.WCH], BF16).ap() for i in range(2)]
        self.cache_mode = None
        self.cache_off = 0
        sb = nc.alloc_sbuf_tensor
        self.XT = sb("XT", [128, 16, ST], BF16).ap()
        self.M = sb("M", [128, 25600], F32).ap()
        self.Wring = sb("Wring", [128, WUNIT * NWUNIT], BF16).ap()
        self.LNG = sb("LNG", [128, D], F32).ap()
        self.LNB = sb("LNB", [128, D], F32).ap()
        self.identb = sb("identb", [128, 128], BF16).ap()
        self.identf = sb("identf", [128, 128], F32).ap()
        self.onesb = sb("onesb", [128, 128], BF16).ap()
        self.onesf = sb("onesf", [128, 128], F32).ap()
        self.ND = sb("ND", [128, 2, 128], F32).ap()
        self.es2 = sb("es2", [128, NL, 8], F32).ap()
        self.dw = sb("dw", [128, NL, 8, 31], F32).ap()
        self.dwb = sb("dwb", [128, NL, 8], F32).ap()
        self.cg = sb("cg", [128, NL, 8], F32).ap()
        self.cb = sb("cb", [128, NL, 8], F32).ap()
        self.Wr = sb("Wr", [128, 16, 36], BF16).ap()
        self.rb = sb("rb", [128, 36], F32).ap()
        self.kcarry = sb("kcarry", [128, 4, 128], BF16).ap()
        self.vcarry = sb("vcarry", [128, 256], BF16).ap()
        self.hcarry = sb("hcarry", [128, 8, 32], BF16).ap()
        self.small = sb("small", [128, 64], F32).ap()
        self.stage = self.mview(65536, [128, 1024], F32)
        self.PS = [nc.alloc_psum_tensor("PS%d" % i, [128, 512], F32).ap() for i in range(8)]
        self.wpos = 0

    def mview(self, off_bytes, shape, dtype):
        assert off_bytes % 4 == 0
        n = int(np.prod(shape[1:]))
        esz = 2 if dtype == BF16 else 4
        nf32 = (n * esz + 3) // 4
        v = self.M[:, off_bytes // 4: off_bytes // 4 + nf32]
        if dtype != F32:
            v = v.bitcast(dtype)
        v = v[:, 0:n]
        if len(shape) == 2:
            return v
        names = " ".join("d%d" % i for i in range(1, len(shape)))
        kw = {"d%d" % i: shape[i] for i in range(1, len(shape))}
        return v.rearrange("p (%s) -> p %s" % (names, names), **kw)

    def walloc(self, kc, ncols):
        n = kc * ncols
        nu = (n + WUNIT - 1) // WUNIT
        if self.wpos + nu > NWUNIT:
            self.wpos = 0
        u0 = self.wpos
        self.wpos += nu
        view = self.Wring[:, u0 * WUNIT: u0 * WUNIT + n].rearrange("p (k n) -> p k n", k=kc)
        return view, [("W", u) for u in range(u0, u0 + nu)]

    def wcache(self, view, regs, n, loaders):
        flat = view.rearrange("p k n -> p (k n)")
        if self.cache_off < self.WCH and self.cache_off + n > self.WCH:
            self.cache_off = self.WCH
        off = self.cache_off
        if self.cache_mode is not None:
            self.cache_off += n
            assert self.cache_off <= self.WCTOT
        wi_ = off // self.WCH
        img = self.WCs[wi_][:, off - wi_ * self.WCH: off - wi_ * self.WCH + n]
        if self.cache_mode == "use":
            self.S.op("sp", lambda e: e.dma_start(out=flat, in_=img), reads=[("WC", off)], writes=regs, dma=True)
            return
        for fn in loaders:
            self.S.op("pool", fn, writes=regs, dma=True)
        if self.cache_mode == "fill":
            self.S.op("sp", lambda e: e.dma_start(out=img, in_=flat), reads=regs, writes=[("WC", off)], dma=True)

    def load_w(self, src, kc, ncols):
        view, regs = self.walloc(kc, ncols)
        self.wcache(view, regs, kc * ncols,
                    [lambda e: e.dma_start(out=view, in_=src.rearrange("(k p) n -> p k n", p=128))])
        return view, regs

    def mm(self, out, pairs, reads, writes):
        def f(e):
            n = len(pairs)
            ins = None
            for i, (l, r) in enumerate(pairs):
                ins = e.matmul(out, l, r, start=(i == 0), stop=(i == n - 1))
            return ins
        return self.S.op("pe", f, reads=reads, writes=writes)

    def dbg(self, name, src, reads):
        if name in self.dbg_d:
            self.S.op("sp", lambda e: e.dma_start(out=self.dbg_d[name], in_=src), reads=reads,
                      writes=[("dbg", name)], dma=True)

    def consts(self):
        S = self.S
        identf, identb = self.identf, self.identb
        S.op("pool", lambda e: e.memset(identf, 0.0), writes=["identf"])
        S.op("pool", lambda e: e.affine_select(identf, identf, [[-1, 128]], ALU.not_equal, 1.0, base=0,
                                               channel_multiplier=1), reads=["identf"], writes=["identf"])
        S.op("dve", lambda e: e.tensor_copy(identb, identf), reads=["identf"], writes=["identb"])
        S.op("dve", lambda e: e.memset(self.onesb, 1.0), writes=["onesb"])
        S.op("dve", lambda e: e.memset(self.onesf, 1.0), writes=["onesf"])
        vi = self.mview(0, [128, 128], I32)
        vf = self.mview(1024, [128, 128], F32)
        vf2 = self.mview(2048, [128, 128], F32)
        S.op("pool", lambda e: e.iota(vi, [[-1, 128]], base=0, channel_multiplier=1), writes=["vi"])
        S.op("dve", lambda e: e.tensor_copy(vf, vi), reads=["vi"], writes=["vf"])
        S.op("dve", lambda e: e.tensor_scalar_add(vf2, vf, -128.0), reads=["vf"], writes=["vf2"])
        ND = self.ND
        S.op("pool", lambda e: e.affine_select(ND[:, 1, :], vf, [[1, 128]], ALU.is_ge, -1e9, base=0,
                                               channel_multiplier=-1), reads=["vf"], writes=["ND1"])
        S.op("pool", lambda e: e.affine_select(ND[:, 0, :], vf2, [[-1, 128]], ALU.is_ge, -1e9, base=-1,
                                               channel_multiplier=1), reads=["vf2"], writes=["ND0"])
        NL = self.NL
        stage = self.stage
        for l in range(NL):
            sk = self.small[:, 0:16]
            S.op("sp", lambda e, l=l: e.dma_start(out=sk, in_=self.p["attn_sinks"][l:l + 1, :].partition_broadcast(128)),
                 writes=["sk"], dma=True)
            S.op("act", lambda e: e.activation(sk, sk, AF.Exp), reads=["sk"], writes=["sk"])
            skv = sk.rearrange("p (c two) -> p c two", two=2)
            S.op("dve", lambda e, l=l: e.tensor_copy(self.es2[0:64, l, :], skv[0:64, :, 0]), reads=["sk"], writes=[("es2", l, 0)])
            S.op("dve", lambda e, l=l: e.tensor_copy(self.es2[64:128, l, :], skv[64:128, :, 1]), reads=["sk"], writes=[("es2", l, 1)])
            S.op("sp", lambda e, l=l: e.dma_start(out=stage[0:31, :], in_=self.p["conv_dw"][l]), writes=["stage"], dma=True)
            pt = self.PS[0][:, 0:248].rearrange("p (c j) -> p c j", c=8)

            def tr(e):
                ins = None
                for c in range(8):
                    ins = e.transpose(pt[:, c, :], stage[0:31, c * 128:(c + 1) * 128], identf[0:31, 0:31])
                return ins
            S.op("pe", tr, reads=["stage", "identf"], writes=["PS0"])
            S.op("dve", lambda e, l=l: e.tensor_copy(self.dw[:, l, :, :], pt), reads=["PS0"], writes=[("dw", l)])
            for j, (nm, dst) in enumerate((("conv_dw_b", self.dwb), ("conv_ln_g", self.cg), ("conv_ln_b", self.cb))):
                S.op("sp", lambda e, l=l, nm=nm, j=j: e.dma_start(
                    out=stage[32 + 8 * j: 40 + 8 * j, 0:128],
                    in_=self.p[nm][l].rearrange("(c p) -> c p", p=128)), writes=[("stv", j)], dma=True)
            pv = self.PS[1][:, 0:24]
            S.op("pe", lambda e: e.transpose(pv, stage[32:56, 0:128], identf[32:56, 32:56]),
                 reads=[("stv", 0), ("stv", 1), ("stv", 2), "identf"], writes=["PS1"])
            for j, dst in enumerate((self.dwb, self.cg, self.cb)):
                S.op("dve", lambda e, l=l, j=j, dst=dst: e.tensor_copy(dst[:, l, :], pv[:, 8 * j: 8 * j + 8]),
                     reads=["PS1"], writes=[("vec", l, j)])
        S.barrier()

    def load_ln(self, gname, bname, l=None):
        S = self.S
        g = self.p[gname]
        b = self.p[bname]
        gsrc = (g[l:l + 1, :] if l is not None else g.rearrange("(o d) -> o d", o=1)).partition_broadcast(128)
        bsrc = (b[l:l + 1, :] if l is not None else b.rearrange("(o d) -> o d", o=1)).partition_broadcast(128)
        S.op("sp", lambda e: e.dma_start(out=self.LNG, in_=gsrc), writes=["LNG"], dma=True)
        S.op("sp", lambda e: e.dma_start(out=self.LNB, in_=bsrc), writes=["LNB"], dma=True)

    def layer_norm_block(self, xr, key, stats_off, small=False):
        S = self.S
        if small:
            base = self.small[:, (stats_off // 128) * 32:(stats_off // 128) * 32 + 32]
            st = base[:, 0:24].rearrange("p (a b) -> p a b", a=4)
            mv = base[:, 24:26]
            rs = base[:, 26:28]
        else:
            st = self.mview(stats_off, [128, 4, 6], F32)
            mv = self.mview(stats_off + 96, [128, 2], F32)
            rs = self.mview(stats_off + 104, [128, 2], F32)
        skey = ("lnst", stats_off)

        def stats(e):
            ins = None
            for i in range(4):
                ins = e.bn_stats(st[:, i, :], xr[:, i * 512:(i + 1) * 512])
            return ins
        S.op("dve", stats, reads=[key], writes=[skey + (0,)])
        S.op("dve", lambda e: e.bn_aggr(mv, st), reads=[skey + (0,)], writes=[skey + (1,)])
        S.op("dve", lambda e: e.tensor_scalar_add(rs[:, 0:1], mv[:, 1:2], LN_EPS), reads=[skey + (1,)], writes=[skey + (2,)])
        S.op("act", lambda e: e.activation(rs[:, 0:1], rs[:, 0:1], AF.Sqrt), reads=[skey + (2,)], writes=[skey + (2,)])
        S.op("dve", lambda e: e.reciprocal(rs[:, 0:1], rs[:, 0:1]), reads=[skey + (2,)], writes=[skey + (3,)])
        S.op("dve", lambda e: e.tensor_scalar(rs[:, 1:2], mv[:, 0:1], rs[:, 0:1], -1.0, ALU.mult, ALU.mult),
             reads=[skey + (1,), skey + (3,)], writes=[skey + (4,)])
        S.op("act", lambda e: e.activation(xr, xr, AF.Identity, bias=rs[:, 1:2], scale=rs[:, 0:1]),
             reads=[key, skey + (3,), skey + (4,)], writes=[key])
        S.op("dve", lambda e: e.tensor_mul(xr, xr, self.LNG), reads=[key, "LNG"], writes=[key])
        S.op("dve", lambda e: e.tensor_add(xr, xr, self.LNB), reads=[key, "LNB"], writes=[key])

    def transpose_block(self, xb, xb_key, dstT, dst_col0, dst_keys, ps_pair=(4, 5)):
        S = self.S
        for hh in range(2):
            ps = self.PS[ps_pair[hh]].bitcast(BF16)[:, 0:1024].rearrange("p (k t) -> p k t", k=8)
            pkey = "PS%d" % ps_pair[hh]

            def tr(e, hh=hh, ps=ps):
                ins = None
                for k in range(8):
                    kk = hh * 8 + k
                    ins = e.transpose(ps[:, k, :], xb[:, kk * 128:(kk + 1) * 128], self.identb)
                return ins
            S.op("pe", tr, reads=[xb_key, "identb"], writes=[pkey])
            eng = "act" if hh == 0 else "dve"
            dk = dst_keys(hh) if callable(dst_keys) else dst_keys
            if eng == "act":
                S.op("act", lambda e, hh=hh, ps=ps: e.copy(dstT[:, hh * 8:(hh + 1) * 8, dst_col0:dst_col0 + 128], ps),
                     reads=[pkey], writes=dk)
            else:
                S.op("dve", lambda e, hh=hh, ps=ps: e.tensor_copy(dstT[:, hh * 8:(hh + 1) * 8, dst_col0:dst_col0 + 128], ps),
                     reads=[pkey], writes=dk)

    def prologue_mem(self):
        S = self.S
        NS = self.NS
        nmb = NS * 2
        self.load_ln("mem_ln_g", "mem_ln_b")
        memT = self.XT
        for mb in range(nmb):
            xr = self.mview(mb % 2 * 8192, [128, D], F32)
            key = ("memxr", mb % 2)
            S.op("sp", lambda e, mb=mb, xr=xr: e.dma_start(out=xr, in_=self.mem_d[mb * 128:(mb + 1) * 128, :]), writes=[key], dma=True)
            self.layer_norm_block(xr, key, (mb % 2) * 128, small=True)
            xb = self.mview(16384 + (mb % 2) * 4096, [128, D], BF16)
            xkey = ("memxb", mb % 2)
            S.op("act", lambda e, xb=xb, xr=xr: e.copy(xb, xr), reads=[key], writes=[xkey])
            self.transpose_block(xb, xkey, memT, mb * 128, [("XT", mb)])
        ncol = nmb * 128
        for l in range(self.NL):
            kmT = self.mview(32768, [128, 8, ncol], BF16)
            vm = self.mview(32768 + 8192, [128, nmb, 1024], BF16)
            xtk = [("XT", mb) for mb in range(nmb)]
            for ct in range(8):
                wt, wreg = self.load_w(self.p["w_mem_kv"][l][:, ct * 256:(ct + 1) * 256], 16, 256)
                if ct < 4:
                    for half in range(2):
                        c = ct * 2 + half
                        ps = self.PS[c % 2]
                        self.mm(ps[:, 0:ncol], [(wt[:, k, half * 128:(half + 1) * 128], memT[:, k, 0:ncol]) for k in range(16)],
                                reads=wreg + xtk, writes=["PS%d" % (c % 2)])
                        S.op("act", lambda e, c=c, ps=ps: e.copy(kmT[:, c, :], ps[:, 0:ncol]), reads=["PS%d" % (c % 2)], writes=[("kmT", c)])
                else:
                    vc = ct - 4
                    for mb in range(nmb):
                        ps = self.PS[2 + mb % 2]
                        self.mm(ps[:, 0:256], [(memT[:, k, mb * 128:(mb + 1) * 128], wt[:, k, :]) for k in range(16)],
                                reads=wreg + xtk, writes=["PS%d" % (2 + mb % 2)])
                        S.op("dve", lambda e, mb=mb, vc=vc, ps=ps: e.tensor_copy(vm[:, mb, vc * 256:(vc + 1) * 256], ps[:, 0:256]),
                             reads=["PS%d" % (2 + mb % 2)], writes=[("vm", mb, vc)])
            S.op("sp", lambda e, l=l: e.dma_start(out=self.KMd[l], in_=kmT), reads=[("kmT", c) for c in range(8)], writes=[("KMd", l)], dma=True)
            S.op("sp", lambda e, l=l: e.dma_start(out=self.VMd[l], in_=vm), reads=[("vm", mb, vc) for mb in range(nmb) for vc in range(4)],
                 writes=[("VMd", l)], dma=True)
        S.barrier()

    def prologue_xT(self):
        S = self.S
        XTd_v = self.XTd.rearrange("(k p) t -> p k t", p=128)
        for s in range(self.NST):
            for b in range(NBLK):
                row0 = s * ST + b * 128
                xr = self.mview((b % 2) * 8192, [128, D], F32)
                key = ("pxr", b % 2)
                S.op("sp", lambda e, xr=xr, row0=row0: e.dma_start(out=xr, in_=self.x_d[row0:row0 + 128, :]), writes=[key], dma=True)
                xb = self.mview(16384 + (b % 2) * 4096, [128, D], BF16)
                xkey = ("pxb", b % 2)
                S.op("act", lambda e, xb=xb, xr=xr: e.copy(xb, xr), reads=[key], writes=[xkey])
                self.transpose_block(xb, xkey, self.XT, b * 128, [("XT", b)])
            S.op("sp", lambda e, s=s: e.dma_start(out=XTd_v[:, :, s * ST:(s + 1) * ST], in_=self.XT),
                 reads=[("XT", b) for b in range(NBLK)], writes=[("XTd", s)], dma=True)
        S.barrier()

    def load_XT(self, s):
        XTd_v = self.XTd.rearrange("(k p) t -> p k t", p=128)
        for hh in range(2):
            self.S.op("sp", lambda e, hh=hh: e.dma_start(out=self.XT[:, hh * 8:(hh + 1) * 8, :],
                                                         in_=XTd_v[:, hh * 8:(hh + 1) * 8, s * ST:(s + 1) * ST]),
                      reads=[("XTd", s)], writes=[("XTh", hh)], dma=True)
        self.xt_keys = [("XTh", 0), ("XTh", 1)]

    def proj_fm(self, l, col0, ntile, evac, ps_ids=(0, 1), wsrc=None):
        w = self.p["w_in"][l] if wsrc is None else wsrc
        i = 0
        for ct in range(ntile):
            wt, wreg = self.load_w(w[:, col0 + ct * 256: col0 + (ct + 1) * 256], 16, 256)
            for half in range(2):
                c = ct * 2 + half
                for tt in range(2):
                    pid = ps_ids[i % len(ps_ids)]
                    i += 1
                    ps = self.PS[pid]
                    self.mm(ps, [(wt[:, k, half * 128:(half + 1) * 128], self.XT[:, k, tt * 512:(tt + 1) * 512]) for k in range(16)],
                            reads=wreg + self.xt_keys, writes=["PS%d" % pid])
                    evac(c, tt, ps, "PS%d" % pid)

    def phase_B(self, l, s):
        S = self.S
        seq_first = (s % (SEQ // ST) == 0)
        h = self.mview(0, [128, 8, 1056], BF16)
        diag = [self.mview(16896 + i * 7936, [128, 31, 128], BF16) for i in range(2)]
        sgt = [self.mview(32768 + i * 2048, [128, 512], F32) for i in range(2)]
        sqt = [self.mview(36864 + i * 2048, [128, 512], F32) for i in range(2)]
        stt = [[self.mview(40960 + (tt * 3 + j) * 2048, [128, 512], F32) for j in range(3)] for tt in range(2)]
        cv = self.mview(53248, [128, 8, ST], F32)
        oc = self.mview(86016, [128, 8, ST], BF16)
        if seq_first:
            S.op("dve", lambda e: e.memset(h[:, :, 0:32], 0.0), writes=[("hh", c) for c in range(8)])
        else:
            S.op("dve", lambda e: e.tensor_copy(h[:, :, 0:32], self.hcarry), reads=["hcarry"], writes=[("hh", c) for c in range(8)])
        wi = self.p["w_in"][l]
        cnt = 0
        for pc in range(4):
            wa, wareg = self.load_w(wi[:, UA0 + pc * 256: UA0 + (pc + 1) * 256], 16, 256)
            wg, wgreg = self.load_w(wi[:, UG0 + pc * 256: UG0 + (pc + 1) * 256], 16, 256)
            for half in range(2):
                c = pc * 2 + half
                for tt in range(2):
                    pa, pg = cnt % 2, 2 + cnt % 2
                    sg = sgt[cnt % 2]
                    sgk = ("sgt", cnt % 2)
                    cnt += 1
                    xs = self.XT
                    self.mm(self.PS[pa], [(wa[:, k, half * 128:(half + 1) * 128], xs[:, k, tt * 512:(tt + 1) * 512]) for k in range(16)],
                            reads=wareg + self.xt_keys, writes=["PS%d" % pa])
                    self.mm(self.PS[pg], [(wg[:, k, half * 128:(half + 1) * 128], xs[:, k, tt * 512:(tt + 1) * 512]) for k in range(16)],
                            reads=wgreg + self.xt_keys, writes=["PS%d" % pg])
                    S.op("act", lambda e, sg=sg, pg=pg: e.activation(sg, self.PS[pg], AF.Sigmoid), reads=["PS%d" % pg], writes=[sgk])
                    S.op("dve", lambda e, sg=sg, pa=pa, c=c, tt=tt: e.tensor_tensor(h[:, c, 32 + tt * 512: 32 + (tt + 1) * 512], self.PS[pa], sg, ALU.mult),
                         reads=["PS%d" % pa, sgk], writes=[("h", c, tt)])
        S.op("dve", lambda e: e.tensor_copy(self.hcarry, h[:, :, 1024:1056]), reads=[("h", c, 1) for c in range(8)], writes=["hcarry"])
        cnt = 0
        for c in range(8):
            dg = diag[c % 2]
            dgk = ("diag", c % 2)
            S.op("pool", lambda e, dg=dg, c=c: e.tensor_tensor(dg, self.identb.unsqueeze(1).to_broadcast([128, 31, 128]),
                                                              self.dw[:, l, c, :].unsqueeze(2).to_broadcast([128, 31, 128]), ALU.mult),
                 reads=["identb", ("dw", l)], writes=[dgk])
            for tt in range(2):
                pid = 4 + cnt % 2
                sq = sqt[cnt % 2]
                sqk = ("sqt", cnt % 2)
                cnt += 1
                rd = [dgk, ("hh", c), ("h", c, tt)] + ([("h", c, 0)] if tt == 1 else [])
                self.mm(self.PS[pid], [(dg[:, j, :], h[:, c, 2 + j + tt * 512: 2 + j + tt * 512 + 512]) for j in range(31)],
                        reads=rd, writes=["PS%d" % pid])
                cvs = cv[:, c, tt * 512:(tt + 1) * 512]
                S.op("act", lambda e, cvs=cvs, pid=pid, c=c: e.activation(cvs, self.PS[pid], AF.Identity, bias=self.dwb[:, l, c:c + 1]),
                     reads=["PS%d" % pid, ("vec", l, 0)], writes=[("cv", c, tt)])
                S.op("pool", lambda e, cvs=cvs, sq=sq: e.tensor_tensor(sq, cvs, cvs, ALU.mult), reads=[("cv", c, tt)], writes=[sqk])
                S.op("pe", lambda e, cvs=cvs, tt=tt, c=c: e.matmul(self.PS[tt * 2], self.onesf, cvs, start=(c == 0), stop=(c == 7)),
                     reads=[("cv", c, tt), "onesf"], writes=["PS%d" % (tt * 2)])
                S.op("pe", lambda e, sq=sq, tt=tt, c=c: e.matmul(self.PS[tt * 2 + 1], self.onesf, sq, start=(c == 0), stop=(c == 7)),
                     reads=[sqk, "onesf"], writes=["PS%d" % (tt * 2 + 1)])
        for tt in range(2):
            mean, rstd, tmp = stt[tt]
            k0 = ("stt", tt)
            S.op("dve", lambda e, mean=mean, tt=tt: e.tensor_scalar_mul(mean, self.PS[tt * 2], 1.0 / 1024), reads=["PS%d" % (tt * 2)], writes=[k0 + (0,)])
            S.op("dve", lambda e, tmp=tmp, mean=mean: e.tensor_tensor(tmp, mean, mean, ALU.mult), reads=[k0 + (0,)], writes=[k0 + (2,)])
            S.op("dve", lambda e, rstd=rstd, tmp=tmp, tt=tt: e.scalar_tensor_tensor(rstd, self.PS[tt * 2 + 1], 1.0 / 1024, tmp, ALU.mult, ALU.subtract),
                 reads=["PS%d" % (tt * 2 + 1), k0 + (2,)], writes=[k0 + (1,)])
            S.op("dve", lambda e, rstd=rstd: e.tensor_scalar_add(rstd, rstd, LN_EPS), reads=[k0 + (1,)], writes=[k0 + (1,)])
            S.op("act", lambda e, rstd=rstd: e.activation(rstd, rstd, AF.Sqrt), reads=[k0 + (1,)], writes=[k0 + (1,)])
            S.op("dve", lambda e, rstd=rstd: e.reciprocal(rstd, rstd), reads=[k0 + (1,)], writes=[k0 + (1,)])
            for c in range(8):
                cvs = cv[:, c, tt * 512:(tt + 1) * 512]
                S.op("dve", lambda e, cvs=cvs, mean=mean: e.tensor_tensor(cvs, cvs, mean, ALU.subtract), reads=[("cv", c, tt), k0 + (0,)], writes=[("cv", c, tt)])
                S.op("pool", lambda e, cvs=cvs, rstd=rstd: e.tensor_tensor(cvs, cvs, rstd, ALU.mult), reads=[("cv", c, tt), k0 + (1,)], writes=[("cv", c, tt)])
                S.op("act", lambda e, cvs=cvs, c=c, tt=tt: e.activation(oc[:, c, tt * 512:(tt + 1) * 512], cvs, AF.Silu,
                                                                        bias=self.cb[:, l, c:c + 1], scale=self.cg[:, l, c:c + 1]),
                     reads=[("cv", c, tt), ("vec", l, 1), ("vec", l, 2)], writes=[("oc", c, tt)])
        if "oc" in self.dbg_d and s == 0 and l == 0:
            S.barrier()
            tmpf = self.mview(0, [128, 8, ST], F32)
            S.op("dve", lambda e: e.tensor_copy(tmpf, oc), writes=["tmpf"])
            self.dbg("oc", tmpf, ["tmpf"])
        S.barrier()

    def phase_A(self, l, s):
        S = self.S
        seq_first = (s % (SEQ // ST) == 0)
        qT = self.mview(0, [128, 8, ST], BF16)
        kd = self.mview(16384, [128, 4, 1152], BF16)
        v = self.mview(25600, [128, 9, 256], BF16)
        tmps = [self.mview(30208 + i * 2048, [128, 2, 2, 128], F32) for i in range(4)]
        pTs = [self.mview(38400 + i * 1024, [128, 2, 2, 128], BF16) for i in range(4)]
        recs = [self.mview(42496 + i * 512, [128, 128], F32) for i in range(4)]
        oa = self.mview(53248, [128, 8, ST], BF16)
        wi = self.p["w_in"][l]
        if not seq_first:
            S.op("dve", lambda e: e.tensor_copy(kd[:, :, 0:128], self.kcarry), reads=["kcarry"], writes=[("kd", g, -1) for g in range(4)])
            S.op("dve", lambda e: e.tensor_copy(v[:, 0, :], self.vcarry), reads=["vcarry"], writes=[("v", -1)])
        def evq(c, tt, ps, pk):
            if (c + tt) % 2 == 0:
                S.op("act", lambda e: e.copy(qT[:, c, tt * 512:(tt + 1) * 512], ps), reads=[pk], writes=[("qT", c, tt * 4 + i) for i in range(4)])
            else:
                S.op("dve", lambda e: e.tensor_copy(qT[:, c, tt * 512:(tt + 1) * 512], ps), reads=[pk], writes=[("qT", c, tt * 4 + i) for i in range(4)])
        self.proj_fm(l, Q0, 4, evq)
        if self.debug.get("substop") == 1:
            S.barrier(); return
        for t in range(2):
            view, wreg = self.walloc(16, 256)
            v5 = view.rearrange("p k (g u d) -> p k g u d", g=2, u=2)
            src = wi[:, K0 + t * 128: K0 + (t + 1) * 128].rearrange("(k p) (g d) -> p k g d", p=128, g=2)
            self.wcache(view, wreg, 16 * 256,
                        [(lambda e, u=u, g=g, v5=v5, src=src: e.dma_start(out=v5[:, :, g, u, :], in_=src[:, :, g, :]))
                         for u in range(2) for g in range(2)])
            for half in range(2):
                g = t * 2 + half
                for tt in range(2):
                    pid = (half * 2 + tt) % 2
                    ps = self.PS[pid]
                    self.mm(ps, [(view[:, k, half * 128:(half + 1) * 128], self.XT[:, k, tt * 512:(tt + 1) * 512]) for k in range(16)],
                            reads=wreg + self.xt_keys, writes=["PS%d" % pid])
                    S.op("act", lambda e, g=g, tt=tt, ps=ps: e.copy(kd[:, g, 128 + tt * 512: 128 + (tt + 1) * 512], ps),
                         reads=["PS%d" % pid], writes=[("kd", g, tt * 4 + i) for i in range(4)])
        S.op("dve", lambda e: e.tensor_copy(self.kcarry, kd[:, :, 1024:1152]), reads=[("kd", g, 7) for g in range(4)], writes=["kcarry"])
        if self.debug.get("substop") == 2:
            S.barrier(); return
        wv, wvreg = self.load_w(wi[:, V0:V0 + 256], 16, 256)
        for b in range(NBLK):
            pid = b % 2
            ps = self.PS[pid]
            self.mm(ps[:, 0:256], [(self.XT[:, k, b * 128:(b + 1) * 128], wv[:, k, :]) for k in range(16)],
                    reads=wvreg + self.xt_keys, writes=["PS%d" % pid])
            S.op("dve", lambda e, b=b, ps=ps: e.tensor_copy(v[:, 1 + b, :], ps[:, 0:256]), reads=["PS%d" % pid], writes=[("v", b)])
        S.op("dve", lambda e: e.tensor_copy(self.vcarry, v[:, 8, :]), reads=[("v", 7)], writes=["vcarry"])
        if self.debug.get("substop") == 3:
            S.barrier(); return
        stages = []
        it = 0
        for b in range(NBLK):
            js = [1] if (seq_first and b == 0) else [0, 1]
            for c in range(8):
                g = c // 2
                i2 = it % 2
                i4 = it % 4
                it += 1
                pss2 = [self.PS[2 * a + i2][:, 0:256].rearrange("p (j t) -> p j t", j=2) for a in range(2)]
                pskeys = ["PS%d" % (2 * a + i2) for a in range(2)]
                pso = self.PS[4 + i4][:, 0:128]
                psd = self.PS[4 + i4][:, 128:256]
                pokey = "PS%d" % (4 + i4)
                tmp, pT, rec = tmps[i4], pTs[i4], recs[i4]
                j0 = js[0]

                def st_sc(b=b, c=c, g=g, js=js, pss2=pss2, pskeys=pskeys):
                    def sc(e):
                        ins = None
                        for j in js:
                            for a in range(2):
                                ins = e.matmul(pss2[a][:, j, :], kd[a * 64:(a + 1) * 64, g, (b + j) * 128:(b + j + 1) * 128],
                                               qT[a * 64:(a + 1) * 64, c, b * 128:(b + 1) * 128], start=True, stop=True)
                        return ins
                    S.op("pe", sc, reads=[("qT", c, b)] + [("kd", g, b - 1 + j) for j in js], writes=pskeys)

                def st_mid(b=b, c=c, g=g, js=js, j0=j0, pss2=pss2, pskeys=pskeys, pso=pso, psd=psd, pokey=pokey, tmp=tmp, pT=pT, i4=i4):
                    for a in range(2):
                        hd = 2 * c + a
                        slope8 = 8.0 * (2.0 ** (-(hd + 1) / 2.0))
                        S.op("dve", lambda e, a=a, slope8=slope8: e.scalar_tensor_tensor(
                            tmp[:, a, j0:2, :], self.ND[:, j0:2, :], slope8, pss2[a][:, j0:2, :], ALU.mult, ALU.add),
                            reads=[pskeys[a], "ND0", "ND1"], writes=[("tmp", i4, a)])
                    S.op("act", lambda e: e.activation(pT[:, :, j0:2, :], tmp[:, :, j0:2, :], AF.Exp, scale=0.125),
                         reads=[("tmp", i4, 0), ("tmp", i4, 1)], writes=[("pT", i4)])

                    def pv(e):
                        ins = None
                        for a in range(2):
                            for ji, j in enumerate(js):
                                e.matmul(pso[a * 64:(a + 1) * 64, :], v[:, b + j, g * 64:(g + 1) * 64], pT[:, a, j, :],
                                         start=(ji == 0), stop=(ji == len(js) - 1))
                        for a in range(2):
                            for ji, j in enumerate(js):
                                ins = e.matmul(psd[a * 64:(a + 1) * 64, :], self.onesb[:, 0:64], pT[:, a, j, :],
                                               start=(ji == 0), stop=(ji == len(js) - 1))
                        return ins
                    S.op("pe", pv, reads=[("pT", i4), "onesb"] + [("v", b - 1 + j) for j in js], writes=[pokey])

                def st_ts(c=c, psd=psd, pokey=pokey, rec=rec, i4=i4):
                    S.op("dve", lambda e: e.tensor_scalar(rec, psd, self.es2[:, l, c:c + 1], 1.0, ALU.add, ALU.mult),
                         reads=[pokey, ("es2", l, 0), ("es2", l, 1)], writes=[("rec", i4)])

                def st_rc(rec=rec, i4=i4):
                    S.op("dve", lambda e: e.reciprocal(rec, rec), reads=[("rec", i4)], writes=[("rec", i4)])

                def st_oa(b=b, c=c, pso=pso, pokey=pokey, rec=rec, i4=i4):
                    S.op("dve", lambda e: e.tensor_tensor(oa[:, c, b * 128:(b + 1) * 128], pso, rec, ALU.mult),
                         reads=[pokey, ("rec", i4)], writes=[("oa", c, b)])
                stages.append((st_sc, st_mid, st_ts, st_rc, st_oa))
        N = len(stages)
        for step in range(N + 4):
            for d in range(5):
                i = step - d
                if 0 <= i < N:
                    stages[i][d]()
        if "oa" in self.dbg_d and s == 0 and l == 0:
            S.barrier()
            tmpf = self.mview(0, [128, 8, ST], F32)
            S.op("dve", lambda e: e.tensor_copy(tmpf, oa), writes=["tmpf"])
            self.dbg("oa", tmpf, ["tmpf"])
        S.barrier()

    def phase_C(self, l, s):
        S = self.S
        seq = s // (SEQ // ST)
        qm = self.mview(0, [128, 8, ST], BF16)
        kmT = self.mview(16384, [128, 8, NMEM], BF16)
        vm = self.mview(20480, [128, 2, 1024], BF16)
        pTs = [self.mview(24576 + i * 2048, [128, 2, 512], BF16) for i in range(2)]
        recs = [self.mview(28672 + i * 2048, [128, 512], F32) for i in range(2)]
        om = self.mview(69632, [128, 8, ST], BF16)
        S.op("sp", lambda e: e.dma_start(out=kmT, in_=self.KMd[l][:, :, seq * NMEM:(seq + 1) * NMEM]), reads=[("KMd", l)], writes=["kmT"], dma=True)
        S.op("sp", lambda e: e.dma_start(out=vm, in_=self.VMd[l][:, seq * 2:(seq + 1) * 2, :]), reads=[("VMd", l)], writes=["vm"], dma=True)

        def evq(c, tt, ps, pk):
            if (c + tt) % 2 == 0:
                S.op("act", lambda e: e.copy(qm[:, c, tt * 512:(tt + 1) * 512], ps), reads=[pk], writes=[("qm", c, tt)])
            else:
                S.op("dve", lambda e: e.tensor_copy(qm[:, c, tt * 512:(tt + 1) * 512], ps), reads=[pk], writes=[("qm", c, tt)])
        self.proj_fm(l, QM0, 4, evq)
        it = 0
        for hh in range(4):
            for tt in range(2):
                i2 = it % 2
                it += 1
                pT, rec = pTs[i2], recs[i2]
                for kb in range(2):
                    pid = 2 + kb
                    self.mm(self.PS[pid], [(kmT[:, 2 * hh + d, kb * 128:(kb + 1) * 128], qm[:, 2 * hh + d, tt * 512:(tt + 1) * 512]) for d in range(2)],
                            reads=["kmT", ("qm", 2 * hh, tt), ("qm", 2 * hh + 1, tt)], writes=["PS%d" % pid])
                    S.op("act", lambda e, kb=kb, pid=pid, pT=pT: e.activation(pT[:, kb, :], self.PS[pid], AF.Exp, scale=1.0 / 16.0),
                         reads=["PS%d" % pid], writes=[("pTm", i2, kb)])
                for d in range(2):
                    pid = 4 + d
                    self.mm(self.PS[pid], [(vm[:, kb, hh * 256 + d * 128: hh * 256 + (d + 1) * 128], pT[:, kb, :]) for kb in range(2)],
                            reads=["vm", ("pTm", i2, 0), ("pTm", i2, 1)], writes=["PS%d" % pid])
                self.mm(self.PS[6], [(self.onesb, pT[:, kb, :]) for kb in range(2)],
                        reads=["onesb", ("pTm", i2, 0), ("pTm", i2, 1)], writes=["PS6"])
                S.op("dve", lambda e, rec=rec: e.reciprocal(rec, self.PS[6]), reads=["PS6"], writes=[("recm", i2)])
                for d in range(2):
                    pid = 4 + d
                    S.op("dve", lambda e, d=d, pid=pid, rec=rec, hh=hh, tt=tt: e.tensor_tensor(
                        om[:, 2 * hh + d, tt * 512:(tt + 1) * 512], self.PS[pid], rec, ALU.mult),
                        reads=["PS%d" % pid, ("recm", i2)], writes=[("om", 2 * hh + d, tt)])
        if "om" in self.dbg_d and s == 0 and l == 0:
            S.barrier()
            tmpf = self.mview(0, [128, 8, ST], F32)
            S.op("dve", lambda e: e.tensor_copy(tmpf, om), writes=["tmpf"])
            self.dbg("om", tmpf, ["tmpf"])
        S.barrier()

    def phase_D(self, l, s):
        S = self.S
        mg = self.mview(0, [128, 16, ST], BF16)
        sgs = [self.mview(32768 + i * 2048, [128, 512], F32) for i in range(3)]
        mts = [self.mview(38912 + i * 2048, [128, 512], F32) for i in range(3)]
        O = [self.mview(53248, [128, 8, ST], BF16), self.mview(86016, [128, 8, ST], BF16), self.mview(69632, [128, 8, ST], BF16)]
        wi = self.p["w_in"][l]
        wb = self.p["w_branch"][l]
        for c in range(16):
            gt = [self.load_w(wi[:, G0 + i * D + c * 128: G0 + i * D + (c + 1) * 128], 16, 128) for i in range(3)]
            bt = [self.load_w(wb[i][:, c * 128:(c + 1) * 128], 8, 128) for i in range(3)]
            for tt in range(2):
                for i in range(3):
                    self.mm(self.PS[i], [(gt[i][0][:, k, :], self.XT[:, k, tt * 512:(tt + 1) * 512]) for k in range(16)],
                            reads=gt[i][1] + self.xt_keys, writes=["PS%d" % i])
                for i in range(3):
                    self.mm(self.PS[3 + i], [(bt[i][0][:, k, :], O[i][:, k, tt * 512:(tt + 1) * 512]) for k in range(8)],
                            reads=bt[i][1], writes=["PS%d" % (3 + i)])
                for i in range(3):
                    S.op("act", lambda e, i=i: e.activation(sgs[i], self.PS[i], AF.Sigmoid), reads=["PS%d" % i], writes=[("sgs", i)])
                    S.op("dve", lambda e, i=i: e.tensor_tensor(mts[i], sgs[i], self.PS[3 + i], ALU.mult),
                         reads=[("sgs", i), "PS%d" % (3 + i)], writes=[("mts", i)])
                S.op("dve", lambda e: e.tensor_tensor(mts[0], mts[0], mts[1], ALU.add), reads=[("mts", 0), ("mts", 1)], writes=[("mts", 0)])
                S.op("dve", lambda e, c=c, tt=tt: e.tensor_tensor(mg[:, c, tt * 512:(tt + 1) * 512], mts[0], mts[2], ALU.add),
                     reads=[("mts", 0), ("mts", 2)], writes=[("mg", c, tt)])
        if "mg" in self.dbg_d and s == 0 and l == 0:
            S.barrier()
            for hh in range(2):
                tmpf = self.mview(36864, [128, 8, ST], F32)
                S.op("dve", lambda e, hh=hh: e.tensor_copy(tmpf, mg[:, hh * 8:(hh + 1) * 8, :]), reads=["tmpf"], writes=["tmpf"])
                S.op("sp", lambda e, hh=hh: e.dma_start(out=self.dbg_d["mg"][:, hh * 8:(hh + 1) * 8, :], in_=tmpf), reads=["tmpf"], writes=["tmpf"], dma=True)
        S.barrier()

    def phase_E(self, l, s):
        S = self.S
        mg = self.mview(0, [128, 16, ST], BF16)
        xb = self.mview(32768, [128, D], BF16)
        self.load_ln("ln1_g", "ln1_b", l)
        wo = self.p["w_out"][l]
        src = self.x_d if l == 0 else self.XRd
        xrs = [self.mview(36864 + half * 32768, [128, 4, D], F32) for half in range(2)]

        def load_half(half):
            xr = xrs[half]
            for b4 in range(4):
                row0 = s * ST + (half * 4 + b4) * 128
                S.op("sp", lambda e, b4=b4, row0=row0, xr=xr: e.dma_start(out=xr[:, b4, :], in_=src[row0:row0 + 128, :]),
                     reads=[("XRd", s)], writes=[("xr", half, b4)], dma=True)

        def mm_n(half, n):
            xr = xrs[half]
            wts = [self.load_w(wo[:, n * 512 + q * 256: n * 512 + (q + 1) * 256], 16, 256) for q in range(2)]
            for b4 in range(4):
                blk = half * 4 + b4
                pid = (n * 4 + b4) % 4

                def f(e, blk=blk, pid=pid, wts=wts):
                    ins = None
                    for q in range(2):
                        for k in range(16):
                            ins = e.matmul(self.PS[pid][:, q * 256:(q + 1) * 256], mg[:, k, blk * 128:(blk + 1) * 128],
                                           wts[q][0][:, k, :], start=(k == 0), stop=(k == 15))
                    return ins
                S.op("pe", f, reads=wts[0][1] + wts[1][1], writes=["PS%d" % pid])
                S.op("dve", lambda e, b4=b4, n=n, pid=pid, xr=xr: e.scalar_tensor_tensor(
                    xr[:, b4, n * 512:(n + 1) * 512], xr[:, b4, n * 512:(n + 1) * 512], ALPHA, self.PS[pid], ALU.mult, ALU.add),
                    reads=["PS%d" % pid, ("xr", half, b4)], writes=[("xr", half, b4)])

        def ln_blk(half, b4):
            blk = half * 4 + b4
            key = ("xr", half, b4)
            xrb = xrs[half][:, b4, :]
            self.layer_norm_block(xrb, key, (blk % 2) * 128, small=True)
            if "x1" in self.dbg_d and s == 0 and l == 0:
                S.op("sp", lambda e, blk=blk, xrb=xrb: e.dma_start(out=self.dbg_d["x1"][blk * 128:(blk + 1) * 128, :], in_=xrb),
                     reads=[key], writes=[("dbgx1", blk)], dma=True)

        def tr_blk(half, b4):
            blk = half * 4 + b4
            key = ("xr", half, b4)
            xrb = xrs[half][:, b4, :]
            S.op("act", lambda e, xrb=xrb: e.copy(xb, xrb), reads=[key], writes=["xb"])
            self.transpose_block(xb, "xb", self.XT, blk * 128, lambda hh: [("XTh", hh)])
            S.op("act", lambda e, xrb=xrb: e.mul(xrb, xrb, ALPHA), reads=[key, ("dbgx1", blk)], writes=[key])

        load_half(0)
        load_half(1)
        for n in range(4):
            mm_n(0, n)
        for i in range(4):
            ln_blk(0, i)
            mm_n(1, i)
            tr_blk(0, i)
        for i in range(4):
            ln_blk(1, i)
            tr_blk(1, i)
        S.barrier()

    def phase_F(self, l, s):
        S = self.S
        last = (l == self.NL - 1)
        yacc = self.mview(36864, [128, 8, D], F32)
        hg = [self.mview(i * 8192, [128, 4, ST], BF16) for i in range(2)]
        sts = [self.mview(16384 + i * 2048, [128, 512], F32) for i in range(2)]
        comb = self.mview(20480, [128, 8, 32], F32)
        lg = self.mview(21504, [128, 8, 36], F32)
        em = self.mview(22656, [128, 8, 32], F32)
        ee = self.mview(23680, [128, 8, 32], F32)
        gsh = self.mview(24704, [128, 8, 4], F32)
        ge = self.mview(24832, [128, 8, 4], F32)
        mx = self.mview(24960, [128, 8, 8], F32)
        gmx = self.mview(25216, [128, 8], F32)
        gs = self.mview(25248, [128, 8], F32)
        d21 = self.mview(25280, [128, 8], F32)
        fac = self.mview(25312, [128, 8], F32)
        xb = self.mview(28672, [128, D], BF16)
        xk = [("XTh", 0), ("XTh", 1)]
        Wr, rb = self.Wr, self.rb
        if s == 0:
            S.op("pool", lambda e: e.dma_start(out=Wr[:, :, 0:4], in_=self.p["router_group"][l].rearrange("(k p) n -> p k n", p=128)),
                 writes=["Wr0"], dma=True)
            S.op("pool", lambda e: e.dma_start(out=Wr[:, :, 4:36], in_=self.p["router_expert"][l].rearrange("(k p) n -> p k n", p=128)),
                 writes=["Wr1"], dma=True)
            S.op("sp", lambda e: e.dma_start(out=rb[:, 0:4], in_=self.p["router_group_b"][l:l + 1, :].partition_broadcast(128)), writes=["rb0"], dma=True)
            S.op("sp", lambda e: e.dma_start(out=rb[:, 4:36], in_=self.p["router_expert_b"][l:l + 1, :].partition_broadcast(128)), writes=["rb1"], dma=True)
        self.load_ln("ln2_g", "ln2_b", l)
        psl = self.PS[0][:, 0:288].rearrange("p (b n) -> p b n", b=8)

        def rl(e):
            ins = None
            for b in range(8):
                for k in range(16):
                    ins = e.matmul(psl[:, b, :], self.XT[:, k, b * 128:(b + 1) * 128], Wr[:, k, :], start=(k == 0), stop=(k == 15))
            return ins
        S.op("pe", rl, reads=["Wr0", "Wr1"] + xk, writes=["PS0"])
        S.op("dve", lambda e: e.tensor_tensor(lg, psl, rb.unsqueeze(1).to_broadcast([128, 8, 36]), ALU.add), reads=["PS0", "rb0", "rb1"], writes=["lg"])
        S.op("dve", lambda e: e.tensor_reduce(gmx, lg[:, :, 0:4], AX.X, ALU.max), reads=["lg"], writes=["gmx"])
        S.op("dve", lambda e: e.tensor_tensor(gsh, lg[:, :, 0:4], gmx.unsqueeze(2).to_broadcast([128, 8, 4]), ALU.subtract), reads=["lg", "gmx"], writes=["gsh"])
        S.op("act", lambda e: e.activation(ge, gsh, AF.Exp), reads=["gsh"], writes=["ge"])
        S.op("dve", lambda e: e.tensor_reduce(gs, ge, AX.X, ALU.add), reads=["ge"], writes=["gs"])
        S.op("dve", lambda e: e.tensor_single_scalar(ge, gsh, 0.0, ALU.is_ge), reads=["gsh", "gs"], writes=["goh"])
        S.op("dve", lambda e: e.tensor_scalar(ge, ge, 1.0, 1e30, ALU.subtract, ALU.mult), reads=["goh"], writes=["goh"])
        lge = lg[:, :, 4:36].rearrange("p b (g j) -> p b g j", g=4)
        em4 = em.rearrange("p b (g j) -> p b g j", g=4)
        S.op("dve", lambda e: e.tensor_tensor(em4, lge, ge.unsqueeze(3).to_broadcast([128, 8, 4, 8]), ALU.add), reads=["lg", "goh"], writes=["em"])

        def mx8(e):
            ins = None
            for b in range(8):
                ins = e.max(mx[:, b, :], em[:, b, :])
            return ins
        S.op("dve", mx8, reads=["em"], writes=["mx"])
        S.op("dve", lambda e: e.tensor_tensor(comb, em, mx[:, :, 1:2].to_broadcast([128, 8, 32]), ALU.is_ge), reads=["em", "mx"], writes=["sel"])
        S.op("dve", lambda e: e.tensor_tensor(ee, em, mx[:, :, 0:1].to_broadcast([128, 8, 32]), ALU.subtract), reads=["em", "mx"], writes=["ee"])
        S.op("act", lambda e: e.activation(ee, ee, AF.Exp), reads=["ee"], writes=["ee"])
        S.op("dve", lambda e: e.tensor_tensor(d21, mx[:, :, 1], mx[:, :, 0], ALU.subtract), reads=["mx"], writes=["d21"])
        S.op("act", lambda e: e.activation(d21, d21, AF.Exp), reads=["d21"], writes=["d21"])
        S.op("dve", lambda e: e.scalar_tensor_tensor(fac, d21, 1.0, gs, ALU.add, ALU.mult), reads=["d21", "gs"], writes=["fac"])
        S.op("dve", lambda e: e.reciprocal(fac, fac), reads=["fac"], writes=["fac"])
        S.op("dve", lambda e: e.tensor_tensor(comb, comb, ee, ALU.mult), reads=["sel", "ee"], writes=["sel"])
        S.op("dve", lambda e: e.tensor_tensor(comb, comb, fac.unsqueeze(2).to_broadcast([128, 8, 32]), ALU.mult), reads=["sel", "fac"], writes=["comb"])
        if "comb" in self.dbg_d and s == 0 and l == 0:
            self.dbg("comb", comb, ["comb"])
        w1, w3, w2 = self.p["w1"][l], self.p["w3"][l], self.p["w2"][l]
        cnt = 0
        cnt4 = 0
        nexp = NEXP if self.moe else 0
        for ex in range(nexp):
            hgb = hg[ex % 2]
            for t in range(2):
                w1t, w1r = self.load_w(w1[ex][:, t * 256:(t + 1) * 256], 16, 256)
                w3t, w3r = self.load_w(w3[ex][:, t * 256:(t + 1) * 256], 16, 256)
                for half in range(2):
                    hc = t * 2 + half
                    for tt in range(2):
                        pa, pb = cnt % 2, 2 + cnt % 2
                        st = sts[cnt % 2]
                        stk = ("sts", cnt % 2)
                        cnt += 1
                        self.mm(self.PS[pa], [(w1t[:, k, half * 128:(half + 1) * 128], self.XT[:, k, tt * 512:(tt + 1) * 512]) for k in range(16)],
                                reads=w1r + xk, writes=["PS%d" % pa])
                        self.mm(self.PS[pb], [(w3t[:, k, half * 128:(half + 1) * 128], self.XT[:, k, tt * 512:(tt + 1) * 512]) for k in range(16)],
                                reads=w3r + xk, writes=["PS%d" % pb])
                        S.op("act", lambda e, st=st, pa=pa: e.activation(st, self.PS[pa], AF.Silu), reads=["PS%d" % pa], writes=[stk])
                        S.op("dve", lambda e, st=st, pb=pb, hc=hc, tt=tt, hgb=hgb: e.tensor_tensor(hgb[:, hc, tt * 512:(tt + 1) * 512], st, self.PS[pb], ALU.mult),
                             reads=[stk, "PS%d" % pb], writes=[("hg", ex % 2, hc, tt)])
            for q in range(2):
                w2t, w2r = self.load_w(w2[ex][:, q * 1024:(q + 1) * 1024], 4, 1024)
                for b in range(8):
                    for n2 in range(2):
                        pid = 4 + cnt4 % 4
                        cnt4 += 1
                        self.mm(self.PS[pid], [(hgb[:, hc, b * 128:(b + 1) * 128], w2t[:, hc, n2 * 512:(n2 + 1) * 512]) for hc in range(4)],
                                reads=w2r + [("hg", ex % 2, hc, b // 4) for hc in range(4)], writes=["PS%d" % pid])
                        ys = yacc[:, b, q * 1024 + n2 * 512: q * 1024 + (n2 + 1) * 512]
                        S.op("dve", lambda e, ys=ys, pid=pid, b=b, ex=ex: e.scalar_tensor_tensor(ys, self.PS[pid], comb[:, b, ex:ex + 1], ys, ALU.mult, ALU.add),
                             reads=["PS%d" % pid, "comb", ("yacc", b)], writes=[("yacc", b)])
        dst = self.out_d if last else self.XRd
        for b in range(8):
            key = ("yacc", b)
            yb = yacc[:, b, :]
            self.layer_norm_block(yb, key, (b % 2) * 128, small=True)
            row0 = s * ST + b * 128
            S.op("sp", lambda e, yb=yb, row0=row0: e.dma_start(out=dst[row0:row0 + 128, :], in_=yb), reads=[key], writes=[("XRd", s, b)], dma=True)
            if not last:
                S.op("act", lambda e, yb=yb: e.copy(xb, yb), reads=[key], writes=["xb"])
                self.transpose_block(xb, "xb", self.XT, b * 128, lambda hh: [("XTh", hh)])
        if not last:
            XTd_v = self.XTd.rearrange("(k p) t -> p k t", p=128)
            S.op("sp", lambda e: e.dma_start(out=XTd_v[:, :, s * ST:(s + 1) * ST], in_=self.XT), reads=xk, writes=[("XTd", s)], dma=True)
        S.barrier()

    def build(self, stop=None):
        def fin():
            self.S.emit()
            return self.nc
        self.consts()
        if stop == "consts":
            return fin()
        self.prologue_mem()
        if stop == "mem":
            return fin()
        self.prologue_xT()
        if stop == "xT":
            return fin()
        for l in range(self.NL):
            for s in range(self.NST):
                self.cache_mode = "fill" if s == 0 else "use"
                if self.NST == 1:
                    self.cache_mode = None
                self.cache_off = 0
                self.load_XT(s)
                for nm, ph in (("B", self.phase_B), ("A", self.phase_A), ("C", self.phase_C), ("D", self.phase_D),
                               ("E", self.phase_E), ("F", self.phase_F)):
                    ph(l, s)
                    if stop == nm:
                        return fin()
        return fin()


def make_in_maps(inputs, n_cores, NS, NL):
    x = np.ascontiguousarray(inputs["x"], dtype=np.float32)
    mem = np.ascontiguousarray(inputs["mem"], dtype=np.float32)
    params = {}
    for name, shp in PARAM_SPECS:
        a = np.asarray(inputs[name], dtype=np.float32)
        if shp[0] == "L":
            a = a[:NL]
        params[name] = np.ascontiguousarray(a)
    maps = []
    for c in range(n_cores):
        m = dict(params)
        m["x"] = np.ascontiguousarray(x[c * NS:(c + 1) * NS].reshape(NS * SEQ, D))
        m["mem"] = np.ascontiguousarray(mem[c * NS:(c + 1) * NS].reshape(NS * NMEM, D))
        maps.append(m)
    return maps


def kernel(**inputs):
    n_cores = 8
    NS, NL = 2, 4
    nc = Builder(NS=NS, NL=NL).build()
    in_maps = make_in_maps(inputs, n_cores, NS, NL)
    res = run_bass_kernel_spmd(nc, in_maps, core_ids=list(range(n_cores)))
    outs = [np.asarray(r["out"]).reshape(NS, SEQ, D) for r in res.results]
    return np.concatenate(outs, axis=0).astype(np.float32)
```
